# Optimizing a Trainium2 kernel written in Bass

```python
import math
import jax, jax.numpy as jnp
from jax import lax
import numpy as np

D_MODEL = 1024
BATCH = 2
SEQ = 8192
DEPTH = 1

HEAD_DIM = 64
N_HEADS_FOX = 8
N_HEADS_DIL = 8
D_FOX = N_HEADS_FOX * HEAD_DIM
D_DIL = N_HEADS_DIL * HEAD_DIM
D_MIX = D_FOX + D_DIL
D_PROJ = 3 * D_FOX + 3 * D_DIL + N_HEADS_FOX
DIL_PATTERNS = ((128, 1), (512, 4), (2048, 16))
BLOCK = 128
ROPE_THETA = 10000.0
N_GROUPS = 4
EXPERTS_PER_GROUP = 8
N_EXPERTS = N_GROUPS * EXPERTS_PER_GROUP
TOP_K_INNER = 2
D_EXPERT = 512
EPS = 1e-6
NEG = -1e30
FORGET_BIAS_INIT = 3.0

kernel_name = "hymba_fox_dilated_hmoe_block"


def rmsnorm(x, g):
    xf = x.astype(jnp.float32)
    y = xf * lax.rsqrt(jnp.mean(xf * xf, axis=-1, keepdims=True) + EPS)
    return (y * g.astype(jnp.float32)).astype(x.dtype)


def rope(x, pos):
    dh = x.shape[-1]
    inv_freq = 1.0 / (ROPE_THETA ** (jnp.arange(0, dh, 2, dtype=jnp.float32) / dh))
    ang = pos[:, None] * inv_freq[None, :]
    cos, sin = jnp.cos(ang), jnp.sin(ang)
    xf = x.astype(jnp.float32)
    x1, x2 = xf[..., : dh // 2], xf[..., dh // 2:]
    out = jnp.concatenate([x1 * cos - x2 * sin, x2 * cos + x1 * sin], axis=-1)
    return out.astype(x.dtype)


def forgetting_attention(q, k, v, logf):
    B, H, S, dh = q.shape
    scale = 1.0 / math.sqrt(dh)
    c = jnp.cumsum(logf, axis=-1)
    nb = S // BLOCK
    qb = q.reshape(B, H, nb, BLOCK, dh).transpose(2, 0, 1, 3, 4)
    cb = c.reshape(B, H, nb, BLOCK).transpose(2, 0, 1, 3)
    kpos = jnp.arange(S)

    def one_block(args):
        qi, ci, i = args
        s = jnp.einsum('bhqd,bhkd->bhqk', qi, k).astype(jnp.float32) * scale
        s = s + ci[..., None] - c[:, :, None, :]
        qpos = i * BLOCK + jnp.arange(BLOCK)
        mask = kpos[None, :] <= qpos[:, None]
        s = jnp.where(mask, s, NEG)
        p = jax.nn.softmax(s, axis=-1)
        return jnp.einsum('bhqk,bhkd->bhqd', p.astype(v.dtype), v)

    out = lax.map(one_block, (qb, cb, jnp.arange(nb)))
    return out.transpose(1, 2, 0, 3, 4).reshape(B, H, S, dh)


def dilated_branch(q, k, v, window, dilation):
    B, H, S, dh = q.shape
    scale = 1.0 / math.sqrt(dh)
    wsub = window // dilation
    unit = dilation * BLOCK
    Sp = -(-S // unit) * unit
    L = Sp // dilation
    nb = L // BLOCK
    pad = ((0, 0), (0, 0), (0, Sp - S), (0, 0))

    def split(t):
        t = jnp.pad(t, pad).reshape(B, H, L, dilation, dh).transpose(0, 1, 3, 2, 4)
        return t.reshape(B, H, dilation, nb, BLOCK, dh)

    def with_prev(t):
        prev = jnp.pad(t, ((0, 0), (0, 0), (0, 0), (1, 0), (0, 0), (0, 0)))[:, :, :, :-1]
        return jnp.concatenate([prev, t], axis=4)

    qs = split(q)
    kw = with_prev(split(k))
    vw = with_prev(split(v))
    s = jnp.einsum('bhrnqd,bhrnkd->bhrnqk', qs, kw).astype(jnp.float32) * scale
    ql = jnp.arange(BLOCK)[:, None]
    kl = jnp.arange(2 * BLOCK)[None, :]
    dist = ql + BLOCK - kl
    kpos = jnp.arange(nb)[:, None, None] * BLOCK + kl[None] - BLOCK
    valid = (dist >= 0) & (dist <= wsub) & (kpos >= 0)
    s = jnp.where(valid, s, NEG)
    m = jnp.max(s, axis=-1, keepdims=True)
    p = jnp.exp(s - m)
    l = jnp.sum(p, axis=-1, keepdims=True)
    o = jnp.einsum('bhrnqk,bhrnkd->bhrnqd', (p / l).astype(v.dtype), vw)
    lse = (m + jnp.log(l))[..., 0]
    o = o.reshape(B, H, dilation, L, dh).transpose(0, 1, 3, 2, 4).reshape(B, H, Sp, dh)[:, :, :S]
    lse = lse.reshape(B, H, dilation, L).transpose(0, 1, 3, 2).reshape(B, H, Sp)[:, :, :S]
    return o, lse


def dilated_attention(q, k, v):
    outs, lses = [], []
    for window, dilation in DIL_PATTERNS:
        o, lse = dilated_branch(q, k, v, window, dilation)
        outs.append(o)
        lses.append(lse)
    w = jax.nn.softmax(jnp.stack(lses, axis=0), axis=0)
    o = jnp.sum(w[..., None] * jnp.stack(outs, axis=0).astype(jnp.float32), axis=0)
    return o.astype(q.dtype)


def token_mixer(xn, w_in, b_forget, q_norm_fox, k_norm_fox, q_norm_dil, k_norm_dil,
                out_norm_fox, out_norm_dil, w_out):
    B, S, _ = xn.shape
    proj = xn @ w_in
    o1 = 3 * D_FOX
    o2 = o1 + 3 * D_DIL
    qa, ka, va = jnp.split(proj[..., :o1], 3, axis=-1)
    qb, kb, vb = jnp.split(proj[..., o1:o2], 3, axis=-1)
    fa = proj[..., o2:]

    def heads(t, h):
        return t.reshape(B, S, h, HEAD_DIM).transpose(0, 2, 1, 3)

    qa = rmsnorm(heads(qa, N_HEADS_FOX), q_norm_fox)
    ka = rmsnorm(heads(ka, N_HEADS_FOX), k_norm_fox)
    va = heads(va, N_HEADS_FOX)
    logf = jax.nn.log_sigmoid(fa.astype(jnp.float32) + b_forget.astype(jnp.float32))
    logf = logf.transpose(0, 2, 1)
    oa = forgetting_attention(qa, ka, va, logf)

    pos = jnp.arange(S, dtype=jnp.float32)
    qb = rope(rmsnorm(heads(qb, N_HEADS_DIL), q_norm_dil), pos)
    kb = rope(rmsnorm(heads(kb, N_HEADS_DIL), k_norm_dil), pos)
    vb = heads(vb, N_HEADS_DIL)
    ob = dilated_attention(qb, kb, vb)

    oa = rmsnorm(oa.transpose(0, 2, 1, 3).reshape(B, S, D_FOX), out_norm_fox)
    ob = rmsnorm(ob.transpose(0, 2, 1, 3).reshape(B, S, D_DIL), out_norm_dil)
    return jnp.concatenate([oa, ob], axis=-1) @ w_out


def hierarchical_moe(xn, w_router_group, b_router_group, w_router_expert, b_router_expert,
                     w_gate, w_up, w_down):
    B, S, D = xn.shape
    t = xn.reshape(B * S, D)
    zg = (t @ w_router_group).astype(jnp.float32) + b_router_group.astype(jnp.float32)
    pg = jax.nn.softmax(zg, axis=-1)
    pg_top, g_sel = lax.top_k(pg, 1)
    ze = jnp.einsum('td,gde->tge', t, w_router_expert).astype(jnp.float32)
    ze = ze + b_router_expert.astype(jnp.float32)
    ze_sel = jnp.take_along_axis(ze, g_sel[:, :, None], axis=1)[:, 0]
    v2, i2 = lax.top_k(ze_sel, TOP_K_INNER)
    gates = jax.nn.softmax(v2, axis=-1) * pg_top
    eid = g_sel * EXPERTS_PER_GROUP + i2
    combine = jnp.sum(jax.nn.one_hot(eid, N_EXPERTS, dtype=jnp.float32) * gates[..., None], axis=1)
    y = jnp.zeros((B * S, D), jnp.float32)
    for e in range(N_EXPERTS):
        h = jax.nn.silu(t @ w_gate[e]) * (t @ w_up[e])
        y = y + combine[:, e:e + 1] * (h @ w_down[e]).astype(jnp.float32)
    return y.astype(xn.dtype).reshape(B, S, D)


def setup_inputs(seed: int = 0) -> dict:
    key = jax.random.key(seed)
    ks = jax.random.split(key, 20)
    f32 = jnp.float32

    def nrm(k, shape, scale):
        return jax.random.normal(k, shape, f32) * scale

    def gain(k, shape):
        return 1.0 + 0.02 * jax.random.normal(k, shape, f32)

    return {
        "x": jax.random.normal(ks[0], (BATCH, SEQ, D_MODEL), f32),
        "norm_mix": gain(ks[1], (DEPTH, D_MODEL)),
        "w_in": nrm(ks[2], (DEPTH, D_MODEL, D_PROJ), D_MODEL ** -0.5),
        "b_forget": FORGET_BIAS_INIT + 0.1 * jax.random.normal(ks[3], (DEPTH, N_HEADS_FOX), f32),
        "q_norm_fox": gain(ks[4], (DEPTH, HEAD_DIM)),
        "k_norm_fox": gain(ks[5], (DEPTH, HEAD_DIM)),
        "q_norm_dil": gain(ks[6], (DEPTH, HEAD_DIM)),
        "k_norm_dil": gain(ks[7], (DEPTH, HEAD_DIM)),
        "out_norm_fox": gain(ks[8], (DEPTH, D_FOX)),
        "out_norm_dil": gain(ks[9], (DEPTH, D_DIL)),
        "w_out": nrm(ks[10], (DEPTH, D_MIX, D_MODEL), D_MIX ** -0.5),
        "norm_ffn": gain(ks[11], (DEPTH, D_MODEL)),
        "w_router_group": nrm(ks[12], (DEPTH, D_MODEL, N_GROUPS), D_MODEL ** -0.5),
        "b_router_group": nrm(ks[13], (DEPTH, N_GROUPS), 0.01),
        "w_router_expert": nrm(ks[14], (DEPTH, N_GROUPS, D_MODEL, EXPERTS_PER_GROUP), D_MODEL ** -0.5),
        "b_router_expert": nrm(ks[15], (DEPTH, N_GROUPS, EXPERTS_PER_GROUP), 0.01),
        "w_gate": nrm(ks[16], (DEPTH, N_EXPERTS, D_MODEL, D_EXPERT), D_MODEL ** -0.5),
        "w_up": nrm(ks[17], (DEPTH, N_EXPERTS, D_MODEL, D_EXPERT), D_MODEL ** -0.5),
        "w_down": nrm(ks[18], (DEPTH, N_EXPERTS, D_EXPERT, D_MODEL), D_EXPERT ** -0.5),
    }


def reference(x, norm_mix, w_in, b_forget, q_norm_fox, k_norm_fox, q_norm_dil, k_norm_dil,
              out_norm_fox, out_norm_dil, w_out, norm_ffn, w_router_group, b_router_group,
              w_router_expert, b_router_expert, w_gate, w_up, w_down):
    h = x
    for l in range(DEPTH):
        xn = rmsnorm(h, norm_mix[l])
        h = h + token_mixer(xn, w_in[l], b_forget[l], q_norm_fox[l], k_norm_fox[l],
                            q_norm_dil[l], k_norm_dil[l], out_norm_fox[l], out_norm_dil[l],
                            w_out[l])
        hn = rmsnorm(h, norm_ffn[l])
        h = h + hierarchical_moe(hn, w_router_group[l], b_router_group[l], w_router_expert[l],
                                 b_router_expert[l], w_gate[l], w_up[l], w_down[l])
    return h
```

```python
import numpy as np
import ml_dtypes
from contextlib import ExitStack
import concourse.bass as bass
import concourse.mybir as mybir
from concourse.bass_utils import run_bass_kernel_spmd

F32 = mybir.dt.float32
BF16 = mybir.dt.bfloat16
AF = mybir.ActivationFunctionType
ALU = mybir.AluOpType
AX = mybir.AxisListType

D = 1024
S_LEN = 8192
CH = 512
NCH = 16
EPS = 1e-6
NEGB = -30000.0
NE = 32
NBF = ml_dtypes.bfloat16


def own_chunks(j):
    return [j, 7 - j, 8 + j, 15 - j]


class Buf:
    __slots__ = ("name", "w", "r")

    def __init__(self, name):
        self.name = name
        self.w = {}
        self.r = {}


class Op:
    __slots__ = ("eng", "fn", "deps", "chan", "signal", "value", "idx")


class Sched:
    ENGS = ("pe", "act", "dve", "pool", "sp")

    def __init__(self, nc, es):
        self.nc = nc
        self.es = es
        self.ops = []
        self.bufs = []
        self.cnt = {e: 0 for e in self.ENGS}
        self.ccnt = {}
        self.sems = {e: es.enter_context(nc.semaphore("s_" + e)) for e in self.ENGS}
        self.chan_sems = {}
        self.total = {e: 0 for e in self.ENGS}

    def buf(self, name="b"):
        b = Buf(name)
        self.bufs.append(b)
        return b

    def _add(self, eng, fn, reads, writes, chan=None):
        op = Op()
        op.eng = eng
        op.fn = fn
        op.chan = chan
        op.signal = chan is not None
        op.value = None
        op.idx = len(self.ops)
        key = ("c", chan) if chan is not None else ("e", eng)
        deps = set()
        for b in reads:
            deps.update(b.w.values())
        for b in writes:
            deps.update(b.w.values())
            deps.update(b.r.values())
        if eng == "pe" and chan is None:
            deps = {i for i in deps if not (self.ops[i].eng == "pe" and self.ops[i].chan is None)}
        op.deps = deps
        for i in deps:
            self.ops[i].signal = True
        for b in reads:
            b.r[key] = op.idx
        for b in writes:
            b.w[key] = op.idx
        self.ops.append(op)
        if chan is not None and chan not in self.chan_sems:
            self.chan_sems[chan] = self.es.enter_context(self.nc.semaphore("c_%s" % chan))
        return op

    def op(self, eng, fn, reads=(), writes=()):
        return self._add(eng, fn, reads, writes)

    def dma(self, queue, out, in_, reads=(), writes=(), chan=None):
        return self._add(queue, lambda e: e.dma_start(out=out, in_=in_), reads, writes, chan=chan)

    def flush(self):
        nc = self.nc
        ops = self.ops
        for op in ops:
            if op.chan is not None:
                self.ccnt[op.chan] = self.ccnt.get(op.chan, 0) + 16
                op.value = self.ccnt[op.chan]
            elif op.signal:
                self.cnt[op.eng] += 1
                op.value = self.cnt[op.eng]
        for e, c in self.cnt.items():
            assert c < 60000, (e, c)
        per_eng = {e: [] for e in self.ENGS}
        for op in ops:
            per_eng[op.eng].append(op)
            self.total[op.eng] += 1
        sems, chan_sems = self.sems, self.chan_sems

        def run_engine(ename, h):
            waited = {}
            for op in per_eng[ename]:
                need = {}
                for i in op.deps:
                    d = ops[i]
                    if d.chan is not None:
                        sem, k = chan_sems[d.chan], ("c", d.chan)
                    else:
                        sem, k = sems[d.eng], ("e", d.eng)
                    if need.get(k, (None, 0))[1] < d.value:
                        need[k] = (sem, d.value)
                for k, (sem, v) in need.items():
                    if waited.get(k, 0) < v:
                        h.wait_ge(sem, v)
                        waited[k] = v
                inst = op.fn(h)
                if op.chan is not None:
                    inst.then_inc(chan_sems[op.chan], 16)
                elif op.signal:
                    inst.then_inc(sems[op.eng], 1)
            last = {}
            for op in per_eng[ename]:
                if op.chan is not None:
                    last[op.chan] = op.value
            for c, v in last.items():
                if waited.get(("c", c), 0) < v:
                    h.wait_ge(chan_sems[c], v)

        with nc.Block() as block:
            @block.tensor
            def _(e):
                run_engine("pe", e)

            @block.scalar
            def _(e):
                run_engine("act", e)

            @block.vector
            def _(e):
                run_engine("dve", e)

            @block.gpsimd
            def _(e):
                run_engine("pool", e)

            @block.sync
            def _(e):
                run_engine("sp", e)
        self.ops = []
        for b in self.bufs:
            b.w.clear()
            b.r.clear()
        self.bufs = []


def build():
    nc = bass.Bass("TRN2", target_bir_lowering=False)

    def din(name, shape, dt=F32):
        return nc.dram_tensor(name, list(shape), dt, kind="ExternalInput").ap()

    xT = din("xT", [NCH, 128, 8, CH])
    xTw = din("xTw", [4, 5, 128, 8, CH])
    xown = din("xown", [2048, D])
    w_in = din("w_in", [128, 8, 3080])
    w_out = din("w_out", [128, 8, D])
    wg = din("wg", [NE, 128, 8, 512])
    wu = din("wu", [NE, 128, 8, 512])
    wd = din("wd", [NE, 128, 4, D])
    wr = din("wr", [128, 8, 36])
    gvec = din("gvec", [128, 28])
    bfv = din("bfv", [8, 1])
    brv = din("brv", [128, 36])
    cbf = din("cbf", [128, 512], BF16)
    fmaskd = din("fmask", [128, 4, 512], BF16)
    dmaskd = din("dmask", [128, 10, 512], BF16)
    identf = din("identf", [128, 128])
    cosw = din("cosw", [4, 128, 5 * CH])
    sinw = din("sinw", [4, 128, 5 * CH])
    tabs = din("tabs", [128, 352])
    oh8d = din("oh8", [8, 64])
    y = nc.dram_tensor("y", [2048, D], F32, kind="ExternalOutput").ap()
    Kd = nc.dram_tensor("Kd", [4, 128, S_LEN], BF16, kind="Internal").ap()
    Vd = nc.dram_tensor("Vd", [4, 128, 64, 130], BF16, kind="Internal").ap()

    with ExitStack() as es:
        S = Sched(nc, es)

        def sbt(stack, name, shape, dt):
            return stack.enter_context(nc.sbuf_tensor(name, list(shape), dt))

        pbank = [es.enter_context(nc.psum_tensor("pb%d" % i, [128, 512], F32)) for i in range(7)]
        pbb = es.enter_context(nc.psum_tensor("pbb", [128, 1024], BF16))
        pbuf = [None] * 7
        ring = {}
        POOLS = {"all": [0, 1, 2, 3, 4, 5, 6], "r": [6], "raw": [0, 1, 2], "h": [3, 4], "m": [5, 6], "c1": [0, 1, 2, 3], "c2": [4, 5]}

        def new_pbufs():
            for i in range(7):
                pbuf[i] = S.buf("pb%d" % i)
            ring.clear()

        def nbank(pool="m"):
            lst = POOLS[pool]
            k = ring.get(pool, 0)
            ring[pool] = k + 1
            i = lst[k % len(lst)]
            return pbank[i], pbuf[i]


        class Pipe:
            def __init__(self):
                self.pending = []

            def push(self, stages):
                self.pending.append(list(stages))
                self.step()

            def step(self):
                for st in list(self.pending):
                    st.pop(0)()
                self.pending = [st for st in self.pending if st]

            def drain(self):
                while self.pending:
                    self.step()

        def mm(out, lhsT, rhs, start, stop, R, W):
            S.op("pe", lambda e: e.matmul(out, lhsT, rhs, start=start, stop=stop), R, W)

        def tr(out, in_, ident, R, W):
            S.op("pe", lambda e: e.transpose(out, in_, ident), R, W)

        def act(out, in_, func, R, W, bias=None, scale=None, accum_out=None):
            kw = {}
            if bias is not None:
                kw["bias"] = bias
            if scale is not None:
                kw["scale"] = scale
            if accum_out is not None:
                kw["accum_out"] = accum_out
            S.op("act", lambda e: e.activation(out=out, in_=in_, func=func, **kw), R, W)

        def ts(eng, out, in0, s1, op0, R, W, s2=None, op1=None):
            if op1 is None:
                S.op(eng, lambda e: e.tensor_scalar(out=out, in0=in0, scalar1=s1, scalar2=None, op0=op0), R, W)
            else:
                S.op(eng, lambda e: e.tensor_scalar(out=out, in0=in0, scalar1=s1, scalar2=s2, op0=op0, op1=op1), R, W)

        def tt(eng, out, in0, in1, op, R, W):
            S.op(eng, lambda e: e.tensor_tensor(out=out, in0=in0, in1=in1, op=op), R, W)

        def stt(out, in0, scalar, in1, op0, op1, R, W):
            S.op("dve", lambda e: e.scalar_tensor_tensor(out=out, in0=in0, scalar=scalar, in1=in1, op0=op0, op1=op1), R, W)

        def cp(eng, out, in_, R, W):
            S.op(eng, lambda e: e.tensor_copy(out=out, in_=in_), R, W)

        def recip(out, in_, R, W):
            S.op("dve", lambda e: e.reciprocal(out=out, in_=in_), R, W)

        def memset(eng, ap, val, W):
            S.op(eng, lambda e: e.memset(ap, val), (), W)

        cb = sbt(es, "cb", [128, 512], BF16)
        gv = sbt(es, "gv", [128, 28], F32)
        idf = sbt(es, "idf", [128, 128], F32)
        tab = sbt(es, "tab", [128, 352], F32)
        onesf = sbt(es, "onesf", [128, 128], F32)
        es_bc = ExitStack()
        oreg = sbt(es_bc, "oreg", [128, 16384], BF16)
        Otok = oreg[:, :].rearrange("p (a b) -> p a b", b=1024)
        ssq = sbt(es_bc, "ssq", [128, 16, 16], F32)
        ones_bf = cb[:, 0:128]
        bdiag = cb[:, 128:256]
        RTm = cb[:, 256:384]
        idb = cb[:, 384:512]
        g_mix = lambda c: gv[:, c:c + 1]
        g_ffn = lambda c: gv[:, 8 + c:9 + c]
        g_out = lambda c: gv[:, 16 + c:17 + c]
        g_qk = lambda i: gv[:, 24 + i:25 + i]
        deadf = lambda ti, kt: tab[:, ti * 64 + kt: ti * 64 + kt + 1]
        amask = lambda ti, kr: tab[:, 256 + ti * 4 + kr: 256 + ti * 4 + kr + 1]
        deadd = lambda ti, w: tab[:, 272 + ti * 20 + w: 272 + ti * 20 + w + 1]

        def load_weights(stack, Bc, name, col_ranges, stg, stg_b, Wt, Wb, gain_fn, src, chan):
            halves = [(stg[:, :, 0:128], S.buf(name + "h0"), chan + "a"), (stg[:, :, 128:256], S.buf(name + "h1"), chan + "b")]
            off = 0
            k = 0
            for (c0, c1) in col_ranges:
                pos = c0
                while pos < c1:
                    n = min(128, c1 - pos)
                    sv, sB, ch = halves[k % 2]
                    S.dma("sp", sv[:, :, 0:n], src[:, :, pos:pos + n], (), [sB], chan=ch)
                    for c in range(8):
                        if c % 2 == 0:
                            ts("dve", Wt[:, c, off:off + n], sv[:, c, 0:n], gain_fn(c), ALU.mult, [sB, Bc], [Wb])
                        else:
                            act(Wt[:, c, off:off + n], sv[:, c, 0:n], AF.Copy, [sB, Bc], [Wb], scale=gain_fn(c))
                    off += n
                    pos += n
                    k += 1
            return off

        def chunk_pre(P, src_ap, slot, need_r8, part="ab", cast_eng="act", sq_eng="dve"):
            xf, xb, xsq = P["xf"][slot], P["xb"][slot], P["xsq"][slot]
            xfB, xbB, xsqB = P["xfB"][slot], P["xbB"][slot], P["xsqB"][slot]
            if "a" in part:
                xch = "xf%d" % (slot % P["nxf"])
                S.dma("sp", xf[:, 0:4, :], src_ap[:, 0:4, :], (), [xfB], chan=xch)
                S.dma("sp", xf[:, 4:8, :], src_ap[:, 4:8, :], (), [xfB], chan=xch)
                if cast_eng == "act":
                    act(xb[:, 0:5, :], xf[:, 0:5, :], AF.Copy, [xfB], [xbB])
                    cp("dve", xb[:, 5:8, :], xf[:, 5:8, :], [xfB], [xbB])
                else:
                    for hh in range(4):
                        cp("pool", xb[:, 2 * hh:2 * hh + 2, :], xf[:, 2 * hh:2 * hh + 2, :], [xfB], [xbB])
                if sq_eng == "act":
                    act(xsq[:].rearrange("p c t -> p (c t)"), xf[:].rearrange("p c t -> p (c t)"), AF.Square, [xfB], [xsqB])
                else:
                    for hh in range(2):
                        tt("dve", xsq[:, 4 * hh:4 * hh + 4, :], xf[:, 4 * hh:4 * hh + 4, :], xf[:, 4 * hh:4 * hh + 4, :], ALU.mult,
                           [xfB], [xsqB])
            if "b" not in part:
                return
            bk, bb = nbank()
            for c in range(8):
                mm(bk[:, :], ones_bf, xsq[:, c, :], c == 0, c == 7, [xsqB, P["cB"]], [bb])
            et, etB = P["et"][slot], P["etB"][slot]
            ts("dve", et[:, :], bk[:, :], EPS / 1024.0, ALU.mult, [bb], [etB], s2=EPS * EPS, op1=ALU.add)
            if need_r8:
                r8, r8B = P["r8"][slot], P["r8B"][slot]
                ts("dve", r8[0:8, :], bk[0:8, :], 1.0 / 1024.0, ALU.mult, [bb], [r8B], s2=EPS, op1=ALU.add)
                act(r8[0:8, :], r8[0:8, :], AF.Ln, [r8B], [r8B])
                act(r8[0:8, :], r8[0:8, :], AF.Exp, [r8B], [r8B], scale=-0.5)
            bk2, bb2 = nbank()
            for t4 in range(4):
                for c in range(8):
                    mm(bk2[:, 2 * t4:2 * t4 + 2], xsq[:, c, t4 * 128:(t4 + 1) * 128], cb[:, 0:2], c == 0, c == 7,
                       [xsqB, P["cB"]], [bb2])
            rt, rtB = P["rt"][slot], P["rtB"][slot]
            ts("dve", rt[:, 0:8], bk2[:, 0:8], 1.0 / 1024.0, ALU.mult, [bb2], [rtB], s2=EPS, op1=ALU.add)
            act(rt[:, 0:8], rt[:, 0:8], AF.Ln, [rtB], [rtB])
            act(rt[:, 0:8], rt[:, 0:8], AF.Exp, [rtB], [rtB], scale=-0.5)

        def norm_stages(P, Wt, WB, colofs, xb, xbB, et, etB, gain_ap, rope, out_ap, outB, k, final=None):
            ksq, t, t2, kn = P["nt"][k % 3]
            ksqB, tB, t2B, knB = P["ntB"][k % 3]
            hold = {}

            def s0():
                bk, bb = nbank("raw")
                hold["raw"] = (bk, bb)
                for c in range(8):
                    mm(bk[:, :], Wt[:, c, colofs:colofs + 128], xb[:, c, :], c == 0, c == 7, [WB, xbB], [bb])
                act(ksq[:, :], bk[:, :], AF.Square, [bb], [ksqB])

            def s1():
                bk, bb = hold["raw"]
                b2, bb2 = nbank("h")
                mm(b2[:, :], bdiag, ksq[:, :], True, True, [ksqB, P["cB"]], [bb2])
                stt(t[:, :], b2[:, :], 1.0 / 64.0, et[:, :], ALU.mult, ALU.add, [bb2, etB], [tB])
                act(t[:, :], t[:, :], AF.Ln, [tB], [tB])
                act(t[:, :], t[:, :], AF.Exp, [tB], [tB], scale=-0.5)
                if rope is None:
                    stt(out_ap, bk[:, :], gain_ap, t[:, :], ALU.mult, ALU.mult, [bb, tB, P["cB"]], [outB])
                    if final is not None:
                        final()
                else:
                    stt(kn[:, :], bk[:, :], gain_ap, t[:, :], ALU.mult, ALU.mult, [bb, tB, P["cB"]], [knB])

            def s2():
                cos_ap, sin_ap, csB = rope
                b3, bb3 = nbank("h")
                mm(b3[:, :], RTm, kn[:, :], True, True, [knB, P["cB"]], [bb3])
                tt("pool", t[:, :], kn[:, :], cos_ap, ALU.mult, [knB, csB], [tB])
                tt("dve", t2[:, :], b3[:, :], sin_ap, ALU.mult, [bb3, csB], [t2B])
                tt("pool", out_ap, t[:, :], t2[:, :], ALU.add, [tB, t2B], [outB])
                if final is not None:
                    final()

            return [s0, s1] if rope is None else [s0, s1, s2]

        def alloc_proj(stack, pre, need_r8, nxb=2, nxf=2):
            P = {}
            P["cB"] = constB[0]
            xfl = [sbt(stack, pre + "xf%d" % i, [128, 8, CH], F32) for i in range(nxf)]
            P["xf"] = [xfl[0], xfl[-1]]
            xbl = [sbt(stack, pre + "xb%d" % i, [128, 8, CH], BF16) for i in range(nxb)]
            xsl = [sbt(stack, pre + "xsq%d" % i, [128, 8, CH], BF16) for i in range(nxb)]
            P["xb"] = [xbl[0], xbl[-1]]
            P["xsq"] = [xsl[0], xsl[-1]]
            P["et"] = [sbt(stack, pre + "et%d" % i, [128, CH], F32) for i in range(2)]
            P["rt"] = [sbt(stack, pre + "rt%d" % i, [128, 8], F32) for i in range(2)]
            for nme in ("et", "rt"):
                P[nme + "B"] = [S.buf(pre + nme + "B%d" % i) for i in range(2)]
            xfbl = [S.buf(pre + "xfB%d" % i) for i in range(nxf)]
            P["xfB"] = [xfbl[0], xfbl[-1]]
            P["nxf"] = nxf
            for nme in ("xb", "xsq"):
                bl = [S.buf(pre + nme + "B%d" % i) for i in range(nxb)]
                P[nme + "B"] = [bl[0], bl[-1]]
            if need_r8:
                P["r8"] = [sbt(stack, pre + "r8%d" % i, [8, CH], F32) for i in range(2)]
                P["r8B"] = [S.buf(pre + "r8B%d" % i) for i in range(2)]
            P["nt"] = [(sbt(stack, pre + "ksq%d" % i, [128, CH], BF16), sbt(stack, pre + "t%d" % i, [128, CH], F32),
                        sbt(stack, pre + "t2%d" % i, [128, CH], F32), sbt(stack, pre + "kn%d" % i, [128, CH], BF16))
                       for i in range(3)]
            P["ntB"] = [tuple(S.buf(pre + "ntB%d_%d" % (i, q)) for q in range(4)) for i in range(3)]
            return P

        constB = [None]

        def load_consts():
            constB[0] = S.buf("const")
            cB = constB[0]
            S.dma("sp", cb[:, :], cbf[:, :], (), [cB], chan="c0")
            S.dma("sp", gv[:, :], gvec[:, :], (), [cB], chan="c0")
            S.dma("sp", idf[:, :], identf[:, :], (), [cB], chan="c0")
            S.dma("sp", tab[:, :], tabs[:, :], (), [cB], chan="c0")
            memset("pool", onesf[:, :], 1.0, [cB])

        ucnt = {"n": 0}

        def attention(steps, ptile, ptB, scr, scrB):
            n_steps = len(steps)
            NS = 5
            LA = 3

            def issue_s(n):
                st = steps[n]
                bk, bb = pbank[2 + n % NS], pbuf[2 + n % NS]
                st["sb"] = (bk, bb)
                mm(bk[:, st["qlo"]:512], st["k"], st["q"], True, True, st["kqR"], [bb])

            for n in range(min(LA, n_steps)):
                issue_s(n)
            for n in range(n_steps):
                if n + LA < n_steps:
                    issue_s(n + LA)
                st = steps[n]
                if st["first"]:
                    ucnt["n"] += 1
                u = ucnt["n"] % 2
                ob, obB = pbank[u], pbuf[u]
                bk, bb = st["sb"]
                qlo = st["qlo"]
                pt, pB = ptile[n % 3], ptB[n % 3]
                act(pt[:, qlo:512], bk[:, qlo:512], AF.Exp, [bb] + st["biasR"], [pB], bias=st["bias"], scale=0.125)
                if st["mask"] is not None:
                    st["mask"](pt, pB, qlo)
                for qb in range(qlo // 128, 4):
                    start = bool(st["first"] and qb == 0)
                    last = st["last"](qb)
                    S.op("pe", (lambda o, l, r, a, b: (lambda e: e.matmul(o, l, r, start=a, stop=b, skip_group_check=True)))(
                        ob[:, qb * 65:(qb + 1) * 65], pt[:, qb * 128:(qb + 1) * 128], st["v"], start, last),
                        [pB] + st["vR"], [obB])
                if st["final"]:
                    ti, hglob = st["unit"]
                    ov = ob[:, 0:260].rearrange("p (q e) -> p q e", e=65)
                    rl4 = scr[0][:, 4 * u:4 * u + 4]
                    recip(rl4, ov[:, :, 64], [obB], [scrB])
                    tt("dve", Otok[:, ti * 4:(ti + 1) * 4, hglob * 64:(hglob + 1) * 64], ov[:, :, 0:64],
                       rl4.unsqueeze(2).broadcast_to([128, 4, 64]), ALU.mult, [obB, scrB], [OtokB[0]])

        OtokB = [None]

        new_pbufs()
        load_consts()
        cB = constB[0]
        OtokB[0] = S.buf("Otok")
        with ExitStack() as pw:
            P = alloc_proj(pw, "w", False, nxb=2, nxf=1)
            stg = sbt(pw, "w_stg", [128, 8, 256], F32)
            stgB = S.buf("stgw")
            Ww = sbt(pw, "Ww", [128, 8, 1536], BF16)
            WwB = S.buf("Ww")
            load_weights(pw, cB, "ww", [(1536, 2048), (2048, 2560), (2560, 3072)], stg, stgB, Ww, WwB, g_mix, w_in, "stgw")
            dm = sbt(pw, "dm", [128, 10, 512], BF16)
            S.dma("sp", dm[:].rearrange("p a b -> p (a b)"), dmaskd.rearrange("p a b -> p (a b)"), (), [cB], chan="c0")
            KTw = sbt(pw, "KTw", [128, 4, 5 * CH], BF16)
            KTwB = S.buf("KTw")
            Vw = sbt(pw, "Vw", [128, 20, 8 * 65], BF16)
            VwB = S.buf("Vw")
            memset("pool", Vw[:].rearrange("p a b -> p (a b)"), 1.0, [VwB])
            QTd = sbt(pw, "QTd", [128, 4, CH], BF16)
            QTdB = S.buf("QTd")
            cs = [(sbt(pw, "cos%d" % i, [128, CH], F32), sbt(pw, "sin%d" % i, [128, CH], F32)) for i in range(2)]
            csB = [S.buf("csB%d" % i) for i in range(2)]
            ptile = [sbt(pw, "wpt%d" % i, [128, 512], BF16) for i in range(3)]
            ptB = [S.buf("wptB%d" % i) for i in range(3)]
            scr = (sbt(pw, "wrl", [128, 8], F32), sbt(pw, "wjunk", [128, 64], F32))
            scrB = S.buf("wscr")
            midx = [0, 1, 2, 3] + [4] * 8 + [5, 6, 7, 8, 9] + [9, 9, 9]
            kcount = 0
            def w_pre(g, part):
                ti_, wc_ = g // 5, g % 5
                sl = g % 2
                chunk_pre(P, xTw[ti_, wc_], sl, False, part=part)
                if "a" in part:
                    S.dma("sp", cs[sl][0][:, :], cosw[ti_, :, wc_ * CH:(wc_ + 1) * CH], (), [csB[sl]], chan="cs%d" % sl)
                    S.dma("sp", cs[sl][1][:, :], sinw[ti_, :, wc_ * CH:(wc_ + 1) * CH], (), [csB[sl]], chan="cs%d" % sl)

            w_pre(0, "ab")
            for ti in range(4):
                for wc in range(5):
                    g = ti * 5 + wc
                    slot = g % 2
                    xb, xbB = P["xb"][slot], P["xbB"][slot]
                    et, etB = P["et"][slot], P["etB"][slot]
                    rt, rtB = P["rt"][slot], P["rtB"][slot]
                    rope = (cs[slot][0][:, :], cs[slot][1][:, :], csB[slot])
                    if wc == 0:
                        pipe = Pipe()

                    def vproj(t4, xb=xb, xbB=xbB, rt=rt, rtB=rtB, wc=wc):
                        bk, bb = nbank("m")
                        for c in range(8):
                            mm(bk[:, :], xb[:, c, t4 * 128:(t4 + 1) * 128], Ww[:, c, 1024:1536], c == 0, c == 7, [xbB, WwB], [bb])
                        act(Vw[:, wc * 4 + t4, :].rearrange("p (h e) -> p h e", e=65)[:, :, 0:64],
                            bk[:, :].rearrange("p (h e) -> p h e", e=64), AF.Copy, [bb, rtB], [VwB],
                            scale=rt[:, 2 * t4:2 * t4 + 1])

                    for hp in range(4):
                        if hp == 2 and g + 1 < 20:
                            w_pre(g + 1, "a")
                        pipe.push(norm_stages(P, Ww, WwB, 512 + hp * 128, xb, xbB, et, etB, g_qk(3), rope,
                                              KTw[:, hp, wc * CH:(wc + 1) * CH], KTwB, kcount))
                        kcount += 1
                        vproj(hp)
                    if wc == 4:
                        for hp in range(4):
                            pipe.push(norm_stages(P, Ww, WwB, hp * 128, xb, xbB, et, etB, g_qk(2), rope,
                                                  QTd[:, hp, :], QTdB, kcount))
                            kcount += 1
                    if wc == 4:
                        pipe.drain()
                    if g + 1 < 20:
                        w_pre(g + 1, "b")
                steps = []
                for h in range(8):
                    hp, half = h // 2, h % 2
                    p0 = 64 * half
                    for w in range(20):
                        r = w - 16
                        qlo = max(0, r) * 128
                        mi = midx[w]

                        def mk(pt, pB, qlo, mi=mi, w=w):
                            eng = "dve"
                            tt(eng, pt[:, qlo:512], pt[:, qlo:512], dm[:, mi, 0:512 - qlo], ALU.mult, [pB, cB], [pB])
                        steps.append({
                            "qlo": qlo,
                            "k": KTw[p0:p0 + 64, hp, w * 128:(w + 1) * 128],
                            "q": QTd[p0:p0 + 64, hp, qlo:512],
                            "kqR": [KTwB, QTdB],
                            "bias": deadd(ti, w),
                            "biasR": [cB],
                            "v": Vw[:, w, h * 65:(h + 1) * 65],
                            "vR": [VwB],
                            "first": w == 0,
                            "last": (lambda qb, w=w: w == 16 + qb),
                            "final": w == 19,
                            "unit": (ti, 8 + h),
                            "mask": mk,
                        })
                attention(steps, ptile, ptB, scr, scrB)
            S.flush()

        es_ab = ExitStack()
        QTf = sbt(es_ab, "QTf", [128, 4, 2048], BF16)
        biasT = sbt(es_ab, "biasT", [128, 4, 64, 8], F32)
        new_pbufs()
        load_consts()
        cB = constB[0]
        QTfB = S.buf("QTf")
        biasB = S.buf("biasT")
        KdB = S.buf("Kd")
        VdB = S.buf("Vd")
        with ExitStack() as pa:
            P = alloc_proj(pa, "a", True)
            stg = sbt(pa, "a_stg", [128, 8, 256], F32)
            stgB = S.buf("stg")
            Wa = sbt(pa, "Wa", [128, 8, 1544], BF16)
            WaB = S.buf("Wa")
            load_weights(pa, cB, "wa", [(512, 1024), (1024, 1536), (3072, 3080), (0, 512)], stg, stgB, Wa, WaB,
                         g_mix, w_in, "stg")
            cc = sbt(pa, "cc", [8, 2, CH], F32)
            cTB = S.buf("cT")
            cmid8 = sbt(pa, "cmid8", [8, 16], F32)
            ctok = sbt(pa, "ctok", [128, 64, 8], F32)
            ctokB = S.buf("ctok")
            bf_sb = sbt(pa, "bf_sb", [8, 1], F32)
            oh8 = sbt(pa, "oh8s", [8, 64], F32)
            S.dma("sp", bf_sb[:, :], bfv[:, :], (), [cB], chan="c0")
            S.dma("sp", oh8[:, :], oh8d[:, :], (), [cB], chan="c0")
            ktile = [sbt(pa, "a_kt%d" % i, [128, CH], BF16) for i in range(4)]
            ktB = [S.buf("a_ktB%d" % i) for i in range(4)]
            vsb = [sbt(pa, "a_v%d" % i, [128, 4, 4, 130], BF16) for i in range(2)]
            vsbB = [S.buf("a_vB%d" % i) for i in range(2)]
            for i in range(2):
                memset("pool", vsb[i][:].rearrange("p a b c -> p (a b c)"), 1.0, [vsbB[i]])
            fz = sbt(pa, "a_fz", [8, 3, CH], F32)
            fzB = S.buf("fz")
            kcount = 0
            deferred = []
            def a_pre(kc_, part):
                if kc_ < NCH:
                    chunk_pre(P, xT[kc_], kc_ % 2, True, part=part)
                else:
                    chunk_pre(P, xTw[kc_ - NCH, 4], kc_ % 2, False, part=part)

            a_pre(0, "ab")
            for kc in range(NCH + 4):
                slot = kc % 2
                is_q = kc >= NCH
                if is_q:
                    while deferred:
                        deferred.pop(0)()
                xb, xbB = P["xb"][slot], P["xbB"][slot]
                et, etB = P["et"][slot], P["etB"][slot]
                rt, rtB = P["rt"][slot], P["rtB"][slot]
                if kc == 0:
                    pipe = Pipe()
                if is_q:
                    ti = kc - NCH
                    for hp in range(4):
                        if hp == 2 and kc + 1 < NCH + 4:
                            a_pre(kc + 1, "a")
                        pipe.push(norm_stages(P, Wa, WaB, 1032 + hp * 128, xb, xbB, et, etB, g_qk(0), None,
                                              QTf[:, hp, ti * CH:(ti + 1) * CH], QTfB, kcount))
                        kcount += 1
                    if kc + 1 < NCH + 4:
                        a_pre(kc + 1, "b")
                    else:
                        pipe.drain()
                    continue
                vs = kc % 2

                def vproj(t4, xb=xb, xbB=xbB, rt=rt, rtB=rtB, vs=vs):
                    bk, bb = nbank("m")
                    for c in range(8):
                        mm(bk[:, :], xb[:, c, t4 * 128:(t4 + 1) * 128], Wa[:, c, 512:1024], c == 0, c == 7, [xbB, WaB], [bb])
                    act(vsb[vs][:, :, t4, :].rearrange("p a (h e) -> p a h e", e=65)[:, :, :, 0:64],
                        bk[:, :].rearrange("p (a h e) -> p a h e", a=4, e=64), AF.Copy, [bb, rtB], [vsbB[vs]],
                        scale=rt[:, 2 * t4:2 * t4 + 1])

                for hp in range(4):
                    if hp == 2:
                        while deferred:
                            deferred.pop(0)()
                        if kc + 1 < NCH + 4:
                            a_pre(kc + 1, "a")
                    ks = kcount % 4

                    def spill(hp=hp, ks=ks, kc=kc):
                        S.dma("pool", Kd[hp, :, kc * CH:(kc + 1) * CH], ktile[ks][:, :], [ktB[ks]], [KdB], chan="ks%d" % ks)
                    pipe.push(norm_stages(P, Wa, WaB, hp * 128, xb, xbB, et, etB, g_qk(1), None,
                                          ktile[ks][:, :], ktB[ks], kcount, final=spill))
                    kcount += 1
                    vproj(hp)
                for hp in range(4):
                    S.dma("pool", Vd[hp, :, kc * 4:(kc + 1) * 4, :], vsb[vs][:, hp, :, :],
                          [vsbB[vs]], [VdB], chan="vs%d" % vs)
                bk, bb = nbank()
                for c in range(8):
                    mm(bk[0:8, :], Wa[:, c, 1024:1032], xb[:, c, :], c == 0, c == 7, [WaB, xbB], [bb])
                r8, r8B = P["r8"][slot], P["r8B"][slot]
                z, mneg, na = fz[:, 0, :], fz[:, 1, :], fz[:, 2, :]
                tt("dve", z, bk[0:8, :], r8[0:8, :], ALU.mult, [bb, r8B], [fzB])
                ts("dve", z, z, bf_sb[:, 0:1], ALU.add, [fzB, cB], [fzB])
                ts("dve", mneg, z, 0.0, ALU.min, [fzB], [fzB])
                ts("dve", na, z, -1.0, ALU.mult, [fzB], [fzB])
                tt("dve", na, na, z, ALU.min, [fzB], [fzB])
                act(na, na, AF.Exp, [fzB], [fzB])
                act(na, na, AF.Ln, [fzB], [fzB], bias=1.0)
                tt("dve", z, mneg, na, ALU.subtract, [fzB], [fzB])
                init = 0.0 if kc == 0 else cc[0:8, (kc - 1) % 2, CH - 1:CH]
                S.op("dve", (lambda o, d0, d1, ini: (lambda e: e.tensor_tensor_scan(out=o, data0=d0, data1=d1, initial=ini,
                                                                                     op0=ALU.mult, op1=ALU.add)))(
                    cc[0:8, kc % 2, :], onesf[0:8, 0:1].broadcast_to([8, CH]), z, init), [fzB, cTB, cB], [cTB])
                cp("dve", cmid8[:, kc:kc + 1], cc[0:8, kc % 2, 255:256], [cTB], [cTB])

                def ctok_tr(kc=kc):
                    bk, bb = nbank()
                    for t4 in range(4):
                        tr(bk[:, 8 * t4:8 * t4 + 8], cc[0:8, kc % 2, t4 * 128:(t4 + 1) * 128], idf[0:8, 0:8], [cTB, cB], [bb])
                    cp("dve", ctok[:, kc * 4:(kc + 1) * 4, :].rearrange("p a b -> p (a b)"), bk[:, 0:32], [bb], [ctokB])
                deferred.append(ctok_tr)
                if kc + 1 < NCH + 4:
                    a_pre(kc + 1, "b")
            crw = sbt(pa, "crw", [8, 64], F32)
            crB = S.buf("cr")
            cmid = cmid8[:, :].unsqueeze(1).broadcast_to([8, 4, 16])
            tt("dve", crw[:, :].rearrange("p (a b) -> p a b", b=16), oh8[:, :].rearrange("p (a b) -> p a b", b=16),
               cmid, ALU.mult, [cTB, cB], [crB])
            cr4 = sbt(pa, "cr4", [8, 4], F32)
            S.op("dve", (lambda o, i_: (lambda e: e.tensor_reduce(out=o, in_=i_, axis=AX.X, op=ALU.add)))(
                cr4[:, :], crw[:, :].rearrange("p (a b) -> p a b", b=16)), [crB], [crB])
            rd = sbt(pa, "rd", [8, 4, 8], F32)
            for ti in range(4):
                ts("dve", rd[:, ti, :], idf[0:8, 0:8], cr4[:, ti:ti + 1], ALU.mult, [crB, cB], [crB])
            bk, bb = nbank()
            mm(bk[:, 0:32], onesf[0:8, :], rd[:].rearrange("p a b -> p (a b)"), True, True, [crB, cB], [bb])
            crb = sbt(pa, "crb", [128, 32], F32)
            cp("dve", crb[:, :], bk[:, 0:32], [bb], [crB])
            for ti in range(4):
                crb_b = crb[:, ti * 8:(ti + 1) * 8].unsqueeze(1).broadcast_to([128, 64, 8])
                tt("dve", biasT[:, ti, :, :], crb_b, ctok[:, :, :], ALU.subtract, [crB, ctokB], [biasB])
                dead_b = tab[:, ti * 64:(ti + 1) * 64].unsqueeze(2).broadcast_to([128, 64, 8])
                tt("dve", biasT[:, ti, :, :], biasT[:, ti, :, :], dead_b, ALU.add, [biasB, cB], [biasB])
            S.flush()

        new_pbufs()
        load_consts()
        cB = constB[0]
        OtokB[0] = S.buf("Otok")
        QTfB = S.buf("QTf")
        biasB = S.buf("biasT")
        with ExitStack() as pbk:
            KTp = [sbt(pbk, "KTp%d" % i, [128, S_LEN], BF16) for i in range(2)]
            V2 = [sbt(pbk, "V2%d" % i, [128, 64, 130], BF16) for i in range(2)]
            KTpB = [S.buf("KTpB%d" % i) for i in range(2)]
            V2B = [S.buf("V2B%d" % i) for i in range(2)]
            fm = sbt(pbk, "fm", [128, 4, 512], BF16)
            S.dma("sp", fm[:].rearrange("p a b -> p (a b)"), fmaskd.rearrange("p a b -> p (a b)"), (), [cB], chan="c0")
            ptile = [sbt(pbk, "pt%d" % i, [128, 512], BF16) for i in range(3)]
            ptB = [S.buf("ptB%d" % i) for i in range(3)]
            scr = (sbt(pbk, "rl", [128, 8], F32), sbt(pbk, "junk", [128, 64], F32))
            scrB = S.buf("scr")

            def load_pair(hp):
                s_ = hp % 2
                for q4 in range(4):
                    S.dma("sp", KTp[s_][:, q4 * 2048:(q4 + 1) * 2048], Kd[hp, :, q4 * 2048:(q4 + 1) * 2048], (), [KTpB[s_]],
                          chan="kl%d" % s_)
                    S.dma("sp", V2[s_][:, q4 * 16:(q4 + 1) * 16, :], Vd[hp, :, q4 * 16:(q4 + 1) * 16, :], (), [V2B[s_]],
                          chan="vl%d" % s_)

            steps = []
            load_pair(0)
            for hp in range(4):
                s_ = hp % 2
                for half in range(2):
                    h = hp * 2 + half
                    p0 = 64 * half
                    for ti in range(4):
                        nkt = 16 * (ti + 1)
                        for kt in range(nkt):
                            kcr = kt // 4 - 4 * ti
                            d = kt % 4
                            st = {
                                "qlo": 0,
                                "k": KTp[s_][p0:p0 + 64, kt * 128:(kt + 1) * 128],
                                "q": QTf[p0:p0 + 64, hp, ti * CH:(ti + 1) * CH],
                                "kqR": [KTpB[s_], QTfB],
                                "bias": biasT[:, ti, kt, h:h + 1],
                                "biasR": [biasB],
                                "v": V2[s_][:, kt, half * 65:(half + 1) * 65],
                                "vR": [V2B[s_]],
                                "first": kt == 0,
                                "last": (lambda qb, kt=kt, nkt=nkt: kt == nkt - 1),
                                "final": kt == nkt - 1,
                                "unit": (ti, h),
                                "mask": None,
                            }
                            if kcr >= 0:
                                def mk(pt, pB, qlo, d=d, ti=ti, kcr=kcr):
                                    stt(pt[:, :], fm[:, d, :], amask(ti, kcr), pt[:, :], ALU.max, ALU.mult, [pB, cB], [pB])
                                st["mask"] = mk
                            steps.append(st)
            per_pair = len(steps) // 4
            for hp in range(4):
                if hp + 1 < 4:
                    load_pair(hp + 1)
                attention(steps[hp * per_pair:(hp + 1) * per_pair], ptile, ptB, scr, scrB)
            S.flush()
        es_ab.close()

        new_pbufs()
        load_consts()
        cB = constB[0]
        OtokB[0] = S.buf("Otok")
        with ExitStack() as pc:
            hsb = sbt(pc, "hsb", [128, 16, D], F32)
            hB = [S.buf("hB%d" % i) for i in range(16)]
            hnT = sbt(pc, "hnT", [128, 8, 2048], BF16)
            hnTB = S.buf("hnT")
            comb = sbt(pc, "comb", [128, 16, NE], F32)
            combB = S.buf("comb")
            br_sb = sbt(pc, "br_sb", [128, 36], F32)
            S.dma("sp", br_sb[:, :], brv[:, :], (), [cB], chan="c0")
            with ExitStack() as pcc:
                stg = sbt(pcc, "c_stg", [128, 8, 256], F32)
                stgB = S.buf("stgc")
                Wo = sbt(pcc, "Wo", [128, 8, D], BF16)
                WoB = S.buf("Wo")
                load_weights(pcc, cB, "wo", [(0, 1024)], stg, stgB, Wo, WoB, g_out, w_out, "stgc")
                wrs = sbt(pcc, "wrs", [128, 8, 36], F32)
                S.dma("sp", wrs[:, :, :], wr[:, :, :], (), [cB], chan="c0")
                ro = sbt(pcc, "ro", [128, 32], F32)
                roB = S.buf("ro")
                rjunk = sbt(pcc, "rjunk", [128, 512], BF16)
                rjB = S.buf("rjunk")
                for i in range(16):
                    for g in range(2):
                        act(rjunk[:, :], Otok[:, i, g * 512:(g + 1) * 512], AF.Square, [OtokB[0]], [rjB, roB],
                            accum_out=ro[:, 2 * i + g:2 * i + g + 1])
                ts("dve", ro[:, :], ro[:, :], 1.0 / 512.0, ALU.mult, [roB], [roB], s2=EPS, op1=ALU.add)
                act(ro[:, :], ro[:, :], AF.Ln, [roB], [roB])
                act(ro[:, :], ro[:, :], AF.Exp, [roB], [roB], scale=-0.5)
                for i in range(16):
                    for g in range(2):
                        act(Otok[:, i, g * 512:(g + 1) * 512], Otok[:, i, g * 512:(g + 1) * 512], AF.Copy,
                            [OtokB[0], roB], [OtokB[0]], scale=ro[:, 2 * i + g:2 * i + g + 1])
                oT = [sbt(pcc, "oT%d" % i, [128, 8, 128], BF16) for i in range(2)]
                oTB = [S.buf("oTB%d" % i) for i in range(2)]
                xo = [sbt(pcc, "xo%d" % i, [128, D], F32) for i in range(2)]
                xoB = [S.buf("xoB%d" % i) for i in range(2)]
                hs = [sbt(pcc, "hs%d" % i, [128, D], F32) for i in range(2)]
                hsB_ = [S.buf("hsB%d" % i) for i in range(2)]
                hTf = sbt(pcc, "hTf0", [128, 8, 128], F32)
                hTfB = S.buf("hTfB0")
                rh = sbt(pcc, "rh", [128, 16], F32)
                rhB = [S.buf("rh%d" % i) for i in range(16)]
                lg = sbt(pcc, "lg", [128, 16, 36], F32)
                lgBs = [S.buf("lg%d" % i) for i in range(16)]
                pbbB = S.buf("pbb")

                def c_stages(i):
                    s_ = i % 2

                    def s0():
                        S.dma("sp", xo[s_][:, :], xown[i * 128:(i + 1) * 128, :], (), [xoB[s_]], chan="xo%d" % s_)
                        for c in range(8):
                            tr(pbb[:, c * 128:(c + 1) * 128], Otok[:, i, c * 128:(c + 1) * 128], idb, [OtokB[0], cB], [pbbB])
                        cp("dve", oT[s_][:].rearrange("p a b -> p (a b)"), pbb[:, :], [pbbB], [oTB[s_]])

                    def s1():
                        for hf in range(2):
                            bk, bb = nbank("c1")
                            for c in range(8):
                                mm(bk[:, :], oT[s_][:, c, :], Wo[:, c, hf * 512:(hf + 1) * 512], c == 0, c == 7, [oTB[s_], WoB], [bb])
                            tt("dve", hsb[:, i, hf * 512:(hf + 1) * 512], bk[:, :], xo[s_][:, hf * 512:(hf + 1) * 512], ALU.add,
                               [bb, xoB[s_]], [hB[i]])
                        act(hs[s_][:, :], hsb[:, i, :], AF.Square, [hB[i]], [hsB_[s_], rhB[i]], accum_out=rh[:, i:i + 1])
                        ts("dve", rh[:, i:i + 1], rh[:, i:i + 1], 1.0 / 1024.0, ALU.mult, [rhB[i]], [rhB[i]], s2=EPS, op1=ALU.add)
                        act(rh[:, i:i + 1], rh[:, i:i + 1], AF.Ln, [rhB[i]], [rhB[i]])
                        act(rh[:, i:i + 1], rh[:, i:i + 1], AF.Exp, [rhB[i]], [rhB[i]], scale=-0.5)
                        act(hs[s_][:, :], hsb[:, i, :], AF.Copy, [hB[i], rhB[i]], [hsB_[s_]], scale=rh[:, i:i + 1])

                    def s2():
                        for half8 in range(2):
                            bk, bb = nbank("c2")
                            for c4 in range(4):
                                c = half8 * 4 + c4
                                tr(bk[:, c4 * 128:(c4 + 1) * 128], hs[s_][:, c * 128:(c + 1) * 128], idf[:, :], [hsB_[s_], cB], [bb])
                            for c4 in range(4):
                                c = half8 * 4 + c4
                                ts("dve", hTf[:, c, :], bk[:, c4 * 128:(c4 + 1) * 128], g_ffn(c), ALU.mult, [bb, cB], [hTfB])
                        cp("dve", hnT[:, :, i * 128:(i + 1) * 128], hTf[:, :, :], [hTfB], [hnTB])

                    def s3():
                        bk, bb = nbank("r")
                        for c in range(8):
                            mm(bk[:, 0:36], hTf[:, c, :], wrs[:, c, :], c == 0, c == 7, [hTfB, cB], [bb])
                        tt("dve", lg[:, i, :], bk[:, 0:36], br_sb[:, :], ALU.add, [bb, cB], [lgBs[i]])
                    return [s0, s1, s2, s3]

                pipe = Pipe()
                for i in range(16):
                    pipe.push(c_stages(i))
                pipe.drain()
                lgB = S.buf("lgall")
                S.op("dve", (lambda a: (lambda e: e.tensor_copy(out=a, in_=a)))(lg[:, 0, 0:1]), lgBs, [lgB])
                rw = sbt(pcc, "rw", [128, 16, 64], F32)
                rwB = S.buf("rw")
                zg = lg[:, :, 0:4]
                ze = lg[:, :, 4:36]
                gmax = rw[:, :, 0:1]
                S.op("dve", (lambda o, i_: (lambda e: e.tensor_reduce(out=o, in_=i_, axis=AX.X, op=ALU.max)))(
                    rw[:, :, 0], zg), [lgB], [rwB])
                goh = rw[:, :, 4:8]
                tt("dve", goh, zg, gmax.broadcast_to([128, 16, 4]), ALU.is_equal, [lgB, rwB], [rwB])
                gex = rw[:, :, 8:12]
                tt("dve", gex, zg, gmax.broadcast_to([128, 16, 4]), ALU.subtract, [lgB, rwB], [rwB])
                act(gex, gex, AF.Exp, [rwB], [rwB])
                S.op("dve", (lambda o, i_: (lambda e: e.tensor_reduce(out=o, in_=i_, axis=AX.X, op=ALU.add)))(
                    rw[:, :, 1], gex), [rwB], [rwB])
                pgt = rw[:, :, 1:2]
                recip(pgt, pgt, [rwB], [rwB])
                zs = rw[:, :, 16:24]
                ztmp = rw[:, :, 24:32]
                for g in range(4):
                    gsel = rw[:, :, 4 + g:5 + g].broadcast_to([128, 16, 8])
                    if g == 0:
                        tt("dve", zs, ze[:, :, 0:8], gsel, ALU.mult, [lgB, rwB], [rwB])
                    else:
                        tt("dve", ztmp, ze[:, :, g * 8:(g + 1) * 8], gsel, ALU.mult, [lgB, rwB], [rwB])
                        tt("dve", zs, zs, ztmp, ALU.add, [rwB], [rwB])
                m1 = rw[:, :, 2:3]
                S.op("dve", (lambda o, i_: (lambda e: e.tensor_reduce(out=o, in_=i_, axis=AX.X, op=ALU.max)))(
                    rw[:, :, 2], zs), [rwB], [rwB])
                oh1 = rw[:, :, 32:40]
                tt("dve", oh1, zs, m1.broadcast_to([128, 16, 8]), ALU.is_equal, [rwB], [rwB])
                zm = rw[:, :, 40:48]
                S.op("dve", (lambda o, a, b: (lambda e: e.scalar_tensor_tensor(out=o, in0=a, scalar=-1e30, in1=b, op0=ALU.mult,
                                                                                 op1=ALU.add)))(zm, oh1, zs), [rwB], [rwB])
                m2 = rw[:, :, 3:4]
                S.op("dve", (lambda o, i_: (lambda e: e.tensor_reduce(out=o, in_=i_, axis=AX.X, op=ALU.max)))(
                    rw[:, :, 3], zm), [rwB], [rwB])
                oh2 = rw[:, :, 48:56]
                tt("dve", oh2, zm, m2.broadcast_to([128, 16, 8]), ALU.is_equal, [rwB], [rwB])
                ee = rw[:, :, 12:13]
                tt("dve", ee, m2, m1, ALU.subtract, [rwB], [rwB])
                act(ee, ee, AF.Exp, [rwB], [rwB])
                den = rw[:, :, 13:14]
                ts("dve", den, ee, 1.0, ALU.add, [rwB], [rwB])
                recip(den, den, [rwB], [rwB])
                g1 = rw[:, :, 14:15]
                tt("dve", g1, den, pgt, ALU.mult, [rwB], [rwB])
                g2 = rw[:, :, 15:16]
                tt("dve", g2, g1, ee, ALU.mult, [rwB], [rwB])
                we = rw[:, :, 56:64]
                tt("dve", oh1, oh1, g1.broadcast_to([128, 16, 8]), ALU.mult, [rwB], [rwB])
                tt("dve", oh2, oh2, g2.broadcast_to([128, 16, 8]), ALU.mult, [rwB], [rwB])
                tt("dve", we, oh1, oh2, ALU.add, [rwB], [rwB])
                for g in range(4):
                    gsel = rw[:, :, 4 + g:5 + g].broadcast_to([128, 16, 8])
                    tt("dve", comb[:, :, g * 8:(g + 1) * 8], we, gsel, ALU.mult, [rwB], [combB])
                S.flush()

            new_pbufs()
            load_consts()
            cB = constB[0]
            hB = [S.buf("hB%d" % i) for i in range(16)]
            hnTB = S.buf("hnT")
            combB = S.buf("comb")
            with ExitStack() as pd:
                NST = 6
                stg4 = [sbt(pd, "d_stg%d" % i, [128, 1024], F32) for i in range(NST)]
                stg4B = [S.buf("d_stgB%d" % i) for i in range(NST)]
                wgb = [oreg[:, i * 4096:(i + 1) * 4096].rearrange("p (c n) -> p c n", n=512) for i in range(2)]
                wub = [oreg[:, 8192 + i * 4096:8192 + (i + 1) * 4096].rearrange("p (c n) -> p c n", n=512) for i in range(2)]
                wdb = [sbt(pd, "wdb%d" % i, [128, 4, D], BF16) for i in range(2)]
                wB = [[S.buf("wB%d_%d" % (k, i)) for i in range(2)] for k in range(3)]
                gs = [sbt(pd, "gs%d" % i, [128, 512], BF16) for i in range(2)]
                gsB = [S.buf("gsB%d" % i) for i in range(2)]
                HT = [sbt(pd, "HT%d" % i, [128, 4, 512], BF16) for i in range(2)]
                HTB = [S.buf("HTB%d" % i) for i in range(2)]
                sc = {"i": 0}

                def load_expert(e):
                    s_ = e % 2
                    pieces = []
                    for k, (src, dst) in enumerate(((wg, wgb), (wu, wub))):
                        v = src[e]
                        for q in range(4):
                            pieces.append((v[:, 2 * q:2 * q + 2, :], dst[s_][:, 2 * q:2 * q + 2, :], wB[k][s_], 2, 512))
                    v = wd[e]
                    for q in range(4):
                        pieces.append((v[:, q:q + 1, :], wdb[s_][:, q:q + 1, :], wB[2][s_], 1, 1024))
                    todo = []
                    for (sv, dv, db, a, b) in pieces:
                        def one(sv=sv, dv=dv, db=db, a=a):
                            i = sc["i"] % NST
                            sc["i"] += 1
                            S.dma("sp", stg4[i][:, :].rearrange("p (a b) -> p a b", a=a), sv, (), [stg4B[i]], chan="ds%d" % i)
                            cp("pool", dv, stg4[i][:, :].rearrange("p (a b) -> p a b", a=a), [stg4B[i]], [db])
                        todo.append(one)
                    return todo

                for one in load_expert(0):
                    one()
                ucount = 0
                for e in range(NE):
                    s_ = e % 2
                    todo = load_expert(e + 1) if e + 1 < NE else []
                    for tq in range(4):
                        hs_ = (e * 4 + tq) % 2
                        for fc in range(4):
                            if todo:
                                todo.pop(0)()
                            bg, bgB = nbank("all")
                            for c in range(8):
                                mm(bg[:, :], wgb[s_][:, c, fc * 128:(fc + 1) * 128], hnT[:, c, tq * 512:(tq + 1) * 512], c == 0, c == 7,
                                   [wB[0][s_], hnTB], [bgB])
                            bu, buB = nbank("all")
                            for c in range(8):
                                mm(bu[:, :], wub[s_][:, c, fc * 128:(fc + 1) * 128], hnT[:, c, tq * 512:(tq + 1) * 512], c == 0, c == 7,
                                   [wB[1][s_], hnTB], [buB])
                            g_ = ucount % 2
                            ucount += 1
                            act(gs[g_][:, :], bg[:, :], AF.Silu, [bgB], [gsB[g_]])
                            tt("dve", HT[hs_][:, fc, :], bu[:, :], gs[g_][:, :], ALU.mult, [buB, gsB[g_]], [HTB[hs_]])
                        for t4 in range(4):
                            i = tq * 4 + t4
                            for hf in range(2):
                                by, byB = nbank("all")
                                for fc in range(4):
                                    mm(by[:, :], HT[hs_][:, fc, t4 * 128:(t4 + 1) * 128], wdb[s_][:, fc, hf * 512:(hf + 1) * 512],
                                       fc == 0, fc == 3, [HTB[hs_], wB[2][s_]], [byB])
                                stt(hsb[:, i, hf * 512:(hf + 1) * 512], by[:, :], comb[:, i, e:e + 1], hsb[:, i, hf * 512:(hf + 1) * 512],
                                    ALU.mult, ALU.add, [byB, combB, hB[i]], [hB[i]])
                yB = S.buf("y")
                for i in range(16):
                    S.dma("sp", y[i * 128:(i + 1) * 128, :], hsb[:, i, :], [hB[i]], [yB], chan="yo%d" % (i % 2))
                S.flush()
        es_bc.close()
        print("ops per engine:", S.total, flush=True)
    return nc


def _consts():
    ones = np.ones((128, 128), np.float32)
    bd = np.zeros((128, 128), np.float32)
    bd[:64, :64] = 1.0
    bd[64:, 64:] = 1.0
    RT = np.zeros((128, 128), np.float32)
    for i in range(128):
        if i % 64 < 32:
            RT[i + 32, i] = -1.0
        else:
            RT[i - 32, i] = 1.0
    ident = np.eye(128, dtype=np.float32)
    cbf = np.concatenate([ones, bd, RT, ident], axis=1).astype(NBF)
    k = np.arange(128)[:, None]
    q = np.arange(512)[None, :]
    fmask = np.stack([(q >= 128 * d + k) for d in range(4)], axis=1).astype(np.float32).astype(NBF)

    def count(delta):
        c = ((delta >= 0) & (delta <= 128)).astype(np.float32)
        c += ((delta >= 0) & (delta <= 512) & (delta % 4 == 0)).astype(np.float32)
        c += ((delta >= 0) & (delta <= 2048) & (delta % 16 == 0)).astype(np.float32)
        return c
    rs = [-16, -15, -14, -13, -12, -4, -3, -2, -1, 0]
    dmask = np.stack([count(q - k - 128 * r) for r in rs], axis=1).astype(NBF)
    for r in range(-12, -4):
        assert np.array_equal(count(q - k - 128 * r), count(q - k + 128 * 12))
    return cbf, fmask, dmask, ident


def _rope_tables(pos):
    inv_freq = (1.0 / (np.float32(10000.0) ** (np.arange(0, 64, 2, dtype=np.float32) / np.float32(64)))).astype(np.float32)
    ang = (pos.astype(np.float32)[:, None] * inv_freq[None, :]).astype(np.float32)
    cos = np.cos(ang).astype(np.float32).T
    sin = np.sin(ang).astype(np.float32).T
    cosT = np.concatenate([cos, cos, cos, cos], axis=0)
    sinT = np.concatenate([sin, sin, sin, sin], axis=0)
    return np.ascontiguousarray(cosT), np.ascontiguousarray(sinT)


_NC_CACHE = {}


def kernel(x, norm_mix, w_in, b_forget, q_norm_fox, k_norm_fox, q_norm_dil, k_norm_dil,
           out_norm_fox, out_norm_dil, w_out, norm_ffn, w_router_group, b_router_group,
           w_router_expert, b_router_expert, w_gate, w_up, w_down):
    f = lambda a: np.ascontiguousarray(np.asarray(a, dtype=np.float32))
    x = f(x)
    cbf, fmask, dmask, ident = _consts()
    pc = lambda v: np.ascontiguousarray(f(v).reshape(8, 128).T)
    t2 = lambda v: np.tile(f(v).reshape(64), 2).reshape(128, 1)
    gvec = np.concatenate([pc(norm_mix[0]), pc(norm_ffn[0]),
                           pc(np.concatenate([f(out_norm_fox[0]), f(out_norm_dil[0])])),
                           t2(q_norm_fox[0]), t2(k_norm_fox[0]), t2(q_norm_dil[0]), t2(k_norm_dil[0])], axis=1)
    gvec = np.ascontiguousarray(gvec.astype(np.float32))
    bfv = f(b_forget[0]).reshape(8, 1)
    wr = np.ascontiguousarray(np.concatenate(
        [f(w_router_group[0]), f(w_router_expert[0]).transpose(1, 0, 2).reshape(D, 32)], axis=1))
    brv = np.ascontiguousarray(np.broadcast_to(
        np.concatenate([f(b_router_group[0]).reshape(4), f(b_router_expert[0]).reshape(32)])[None, :], (128, 36)))
    pm = lambda a, c: np.ascontiguousarray(a.reshape(a.shape[:-2] + (c, 128, a.shape[-1])).swapaxes(-3, -2))
    shared = {
        "w_in": pm(f(w_in[0]), 8), "w_out": pm(f(w_out[0]), 8), "wg": pm(f(w_gate[0]), 8), "wu": pm(f(w_up[0]), 8),
        "wd": pm(f(w_down[0]), 4), "wr": pm(wr, 8), "gvec": gvec, "bfv": bfv, "brv": brv, "cbf": cbf, "fmask": fmask, "dmask": dmask, "identf": ident,
    }
    xTs = [np.ascontiguousarray(x[b].T) for b in range(2)]
    cpm = lambda xt: np.ascontiguousarray(xt.reshape(8, 128, -1, CH).transpose(2, 1, 0, 3))
    xTc = [cpm(xTs[b]) for b in range(2)]
    in_maps = []
    for c in range(8):
        b, j = c // 4, c % 4
        own = own_chunks(j)
        xTw = np.zeros((4, D, 5 * CH), np.float32)
        cosw = np.zeros((4, 128, 5 * CH), np.float32)
        sinw = np.zeros((4, 128, 5 * CH), np.float32)
        tabs = np.zeros((128, 352), np.float32)
        oh8 = np.zeros((8, 4, 16), np.float32)
        for ti, ch in enumerate(own):
            lo = (ch - 4) * CH
            pos = np.arange(lo, lo + 5 * CH)
            valid = pos >= 0
            xTw[ti][:, valid] = xTs[b][:, pos[valid]]
            cw, sw = _rope_tables(np.maximum(pos, 0).astype(np.float32))
            cosw[ti], sinw[ti] = cw, sw
            for kt in range(64):
                tabs[:, ti * 64 + kt] = 0.0 if (kt // 4) <= ch else NEGB
            for kr in range(4):
                tabs[:, 256 + ti * 4 + kr] = 0.0 if (4 * ti + kr) == ch else 1.0
            for w in range(20):
                tabs[:, 272 + ti * 20 + w] = 0.0 if (lo + w * 128) >= 0 else NEGB
            oh8[:, ti, ch] = 1.0
        xown = np.concatenate([x[b, ch * CH:(ch + 1) * CH, :] for ch in own], axis=0)
        m = dict(shared)
        m.update({"xT": xTc[b], "xTw": np.stack([cpm(xTw[t_]) for t_ in range(4)]), "xown": np.ascontiguousarray(xown), "cosw": cosw, "sinw": sinw,
                  "tabs": tabs, "oh8": np.ascontiguousarray(oh8.reshape(8, 64))})
        in_maps.append(m)
    if "nc" not in _NC_CACHE:
        _NC_CACHE["nc"] = build()
    nc = _NC_CACHE["nc"]
    res = run_bass_kernel_spmd(nc, in_maps, core_ids=list(range(8)))
    out = np.zeros((2, S_LEN, D), np.float32)
    for c in range(8):
        b, j = c // 4, c % 4
        yc = np.asarray(res.results[c]["y"], dtype=np.float32)
        for ti, ch in enumerate(own_chunks(j)):
            out[b, ch * CH:(ch + 1) * CH, :] = yc[ti * CH:(ti + 1) * CH, :]
    return out
```

```python
import numpy as np
import ml_dtypes
from contextlib import ExitStack
import concourse.bass as bass
import concourse.mybir as mybir
from concourse.bass_utils import run_bass_kernel_spmd

F32 = mybir.dt.float32
BF16 = mybir.dt.bfloat16
AF = mybir.ActivationFunctionType
ALU = mybir.AluOpType
AX = mybir.AxisListType

D = 1024
S_LEN = 8192
CH = 512
NCH = 16
EPS = 1e-6
NEGB = -30000.0
NE = 32
NBF = ml_dtypes.bfloat16


def own_chunks(j):
    return [j, 7 - j, 8 + j, 15 - j]


class Buf:
    __slots__ = ("name", "w", "r")

    def __init__(self, name):
        self.name = name
        self.w = {}
        self.r = {}


class Op:
    __slots__ = ("eng", "fn", "deps", "chan", "signal", "value", "idx")


class Sched:
    ENGS = ("pe", "act", "dve", "pool", "sp")

    def __init__(self, nc, es):
        self.nc = nc
        self.es = es
        self.ops = []
        self.bufs = []
        self.cnt = {e: 0 for e in self.ENGS}
        self.ccnt = {}
        self.sems = {e: es.enter_context(nc.semaphore("s_" + e)) for e in self.ENGS}
        self.chan_sems = {}
        self.total = {e: 0 for e in self.ENGS}

    def buf(self, name="b"):
        b = Buf(name)
        self.bufs.append(b)
        return b

    def _add(self, eng, fn, reads, writes, chan=None):
        op = Op()
        op.eng = eng
        op.fn = fn
        op.chan = chan
        op.signal = chan is not None
        op.value = None
        op.idx = len(self.ops)
        key = ("c", chan) if chan is not None else ("e", eng)
        deps = set()
        for b in reads:
            deps.update(b.w.values())
        for b in writes:
            deps.update(b.w.values())
            deps.update(b.r.values())
        if eng == "pe" and chan is None:
            deps = {i for i in deps if not (self.ops[i].eng == "pe" and self.ops[i].chan is None)}
        op.deps = deps
        for i in deps:
            self.ops[i].signal = True
        for b in reads:
            b.r[key] = op.idx
        for b in writes:
            b.w[key] = op.idx
        self.ops.append(op)
        if chan is not None and chan not in self.chan_sems:
            self.chan_sems[chan] = self.es.enter_context(self.nc.semaphore("c_%s" % chan))
        return op

    def op(self, eng, fn, reads=(), writes=()):
        return self._add(eng, fn, reads, writes)

    def dma(self, queue, out, in_, reads=(), writes=(), chan=None):
        return self._add(queue, lambda e: e.dma_start(out=out, in_=in_), reads, writes, chan=chan)

    def flush(self):
        nc = self.nc
        ops = self.ops
        for op in ops:
            if op.chan is not None:
                self.ccnt[op.chan] = self.ccnt.get(op.chan, 0) + 16
                op.value = self.ccnt[op.chan]
            elif op.signal:
                self.cnt[op.eng] += 1
                op.value = self.cnt[op.eng]
        for e, c in self.cnt.items():
            assert c < 60000, (e, c)
        per_eng = {e: [] for e in self.ENGS}
        for op in ops:
            per_eng[op.eng].append(op)
            self.total[op.eng] += 1
        sems, chan_sems = self.sems, self.chan_sems

        def run_engine(ename, h):
            waited = {}
            for op in per_eng[ename]:
                need = {}
                for i in op.deps:
                    d = ops[i]
                    if d.chan is not None:
                        sem, k = chan_sems[d.chan], ("c", d.chan)
                    else:
                        sem, k = sems[d.eng], ("e", d.eng)
                    if need.get(k, (None, 0))[1] < d.value:
                        need[k] = (sem, d.value)
                for k, (sem, v) in need.items():
                    if waited.get(k, 0) < v:
                        h.wait_ge(sem, v)
                        waited[k] = v
                inst = op.fn(h)
                if op.chan is not None:
                    inst.then_inc(chan_sems[op.chan], 16)
                elif op.signal:
                    inst.then_inc(sems[op.eng], 1)
            last = {}
            for op in per_eng[ename]:
                if op.chan is not None:
                    last[op.chan] = op.value
            for c, v in last.items():
                if waited.get(("c", c), 0) < v:
                    h.wait_ge(chan_sems[c], v)

        with nc.Block() as block:
            @block.tensor
            def _(e):
                run_engine("pe", e)

            @block.scalar
            def _(e):
                run_engine("act", e)

            @block.vector
            def _(e):
                run_engine("dve", e)

            @block.gpsimd
            def _(e):
                run_engine("pool", e)

            @block.sync
            def _(e):
                run_engine("sp", e)
        self.ops = []
        for b in self.bufs:
            b.w.clear()
            b.r.clear()
        self.bufs = []


def build():
    nc = bass.Bass("TRN2", target_bir_lowering=False)

    def din(name, shape, dt=F32):
        return nc.dram_tensor(name, list(shape), dt, kind="ExternalInput").ap()

    xT = din("xT", [NCH, 128, 8, CH])
    xTw = din("xTw", [4, 5, 128, 8, CH])
    xown = din("xown", [2048, D])
    w_in = din("w_in", [128, 8, 3080])
    w_out = din("w_out", [128, 8, D])
    wg = din("wg", [NE, 128, 8, 512])
    wu = din("wu", [NE, 128, 8, 512])
    wd = din("wd", [NE, 128, 4, D])
    wr = din("wr", [128, 8, 36])
    gvec = din("gvec", [128, 28])
    bfv = din("bfv", [8, 1])
    brv = din("brv", [128, 36])
    cbf = din("cbf", [128, 512], BF16)
    fmaskd = din("fmask", [128, 4, 512], BF16)
    dmaskd = din("dmask", [128, 10, 512], BF16)
    identf = din("identf", [128, 128])
    cosw = din("cosw", [4, 128, 5 * CH])
    sinw = din("sinw", [4, 128, 5 * CH])
    tabs = din("tabs", [128, 352])
    oh8d = din("oh8", [8, 64])
    y = nc.dram_tensor("y", [2048, D], F32, kind="ExternalOutput").ap()
    Kd = nc.dram_tensor("Kd", [4, 128, S_LEN], BF16, kind="Internal").ap()
    Vd = nc.dram_tensor("Vd", [4, 128, 64, 130], BF16, kind="Internal").ap()

    with ExitStack() as es:
        S = Sched(nc, es)

        def sbt(stack, name, shape, dt):
            return stack.enter_context(nc.sbuf_tensor(name, list(shape), dt))

        pbank = [es.enter_context(nc.psum_tensor("pb%d" % i, [128, 512], F32)) for i in range(7)]
        pbb = es.enter_context(nc.psum_tensor("pbb", [128, 1024], BF16))
        pbuf = [None] * 7
        ring = {}
        POOLS = {"all": [0, 1, 2, 3, 4, 5, 6], "r": [6], "raw": [0, 1, 2], "h": [3, 4], "m": [5, 6], "c1": [0, 1, 2, 3], "c2": [4, 5]}

        def new_pbufs():
            for i in range(7):
                pbuf[i] = S.buf("pb%d" % i)
            ring.clear()

        def nbank(pool="m"):
            lst = POOLS[pool]
            k = ring.get(pool, 0)
            ring[pool] = k + 1
            i = lst[k % len(lst)]
            return pbank[i], pbuf[i]


        class Pipe:
            def __init__(self):
                self.pending = []

            def push(self, stages):
                self.pending.append(list(stages))
                self.step()

            def step(self):
                for st in list(self.pending):
                    st.pop(0)()
                self.pending = [st for st in self.pending if st]

            def drain(self):
                while self.pending:
                    self.step()

        def mm(out, lhsT, rhs, start, stop, R, W):
            S.op("pe", lambda e: e.matmul(out, lhsT, rhs, start=start, stop=stop), R, W)

        def tr(out, in_, ident, R, W):
            S.op("pe", lambda e: e.transpose(out, in_, ident), R, W)

        def act(out, in_, func, R, W, bias=None, scale=None, accum_out=None):
            kw = {}
            if bias is not None:
                kw["bias"] = bias
            if scale is not None:
                kw["scale"] = scale
            if accum_out is not None:
                kw["accum_out"] = accum_out
            S.op("act", lambda e: e.activation(out=out, in_=in_, func=func, **kw), R, W)

        def ts(eng, out, in0, s1, op0, R, W, s2=None, op1=None):
            if op1 is None:
                S.op(eng, lambda e: e.tensor_scalar(out=out, in0=in0, scalar1=s1, scalar2=None, op0=op0), R, W)
            else:
                S.op(eng, lambda e: e.tensor_scalar(out=out, in0=in0, scalar1=s1, scalar2=s2, op0=op0, op1=op1), R, W)

        def tt(eng, out, in0, in1, op, R, W):
            S.op(eng, lambda e: e.tensor_tensor(out=out, in0=in0, in1=in1, op=op), R, W)

        def stt(out, in0, scalar, in1, op0, op1, R, W):
            S.op("dve", lambda e: e.scalar_tensor_tensor(out=out, in0=in0, scalar=scalar, in1=in1, op0=op0, op1=op1), R, W)

        def cp(eng, out, in_, R, W):
            S.op(eng, lambda e: e.tensor_copy(out=out, in_=in_), R, W)

        def recip(out, in_, R, W):
            S.op("dve", lambda e: e.reciprocal(out=out, in_=in_), R, W)

        def memset(eng, ap, val, W):
            S.op(eng, lambda e: e.memset(ap, val), (), W)

        cb = sbt(es, "cb", [128, 512], BF16)
        gv = sbt(es, "gv", [128, 28], F32)
        idf = sbt(es, "idf", [128, 128], F32)
        tab = sbt(es, "tab", [128, 352], F32)
        onesf = sbt(es, "onesf", [128, 128], F32)
        es_bc = ExitStack()
        oreg = sbt(es_bc, "oreg", [128, 16384], BF16)
        Otok = oreg[:, :].rearrange("p (a b) -> p a b", b=1024)
        ssq = sbt(es_bc, "ssq", [128, 16, 16], F32)
        ones_bf = cb[:, 0:128]
        bdiag = cb[:, 128:256]
        RTm = cb[:, 256:384]
        idb = cb[:, 384:512]
        g_mix = lambda c: gv[:, c:c + 1]
        g_ffn = lambda c: gv[:, 8 + c:9 + c]
        g_out = lambda c: gv[:, 16 + c:17 + c]
        g_qk = lambda i: gv[:, 24 + i:25 + i]
        deadf = lambda ti, kt: tab[:, ti * 64 + kt: ti * 64 + kt + 1]
        amask = lambda ti, kr: tab[:, 256 + ti * 4 + kr: 256 + ti * 4 + kr + 1]
        deadd = lambda ti, w: tab[:, 272 + ti * 20 + w: 272 + ti * 20 + w + 1]

        def load_weights(stack, Bc, name, col_ranges, stg, stg_b, Wt, Wb, gain_fn, src, chan):
            halves = [(stg[:, :, 0:128], S.buf(name + "h0"), chan + "a"), (stg[:, :, 128:256], S.buf(name + "h1"), chan + "b")]
            off = 0
            k = 0
            for (c0, c1) in col_ranges:
                pos = c0
                while pos < c1:
                    n = min(128, c1 - pos)
                    sv, sB, ch = halves[k % 2]
                    S.dma("sp", sv[:, :, 0:n], src[:, :, pos:pos + n], (), [sB], chan=ch)
                    for c in range(8):
                        if c % 2 == 0:
                            ts("dve", Wt[:, c, off:off + n], sv[:, c, 0:n], gain_fn(c), ALU.mult, [sB, Bc], [Wb])
                        else:
                            act(Wt[:, c, off:off + n], sv[:, c, 0:n], AF.Copy, [sB, Bc], [Wb], scale=gain_fn(c))
                    off += n
                    pos += n
                    k += 1
            return off

        def chunk_pre(P, src_ap, slot, need_r8, part="ab", cast_eng="act", sq_eng="dve"):
            xf, xb, xsq = P["xf"][slot], P["xb"][slot], P["xsq"][slot]
            xfB, xbB, xsqB = P["xfB"][slot], P["xbB"][slot], P["xsqB"][slot]
            if "a" in part:
                xch = "xf%d" % (slot % P["nxf"])
                S.dma("sp", xf[:, 0:4, :], src_ap[:, 0:4, :], (), [xfB], chan=xch)
                S.dma("sp", xf[:, 4:8, :], src_ap[:, 4:8, :], (), [xfB], chan=xch)
                if cast_eng == "act":
                    act(xb[:].rearrange("p c t -> p (c t)"), xf[:].rearrange("p c t -> p (c t)"), AF.Copy, [xfB], [xbB])
                else:
                    for hh in range(4):
                        cp("pool", xb[:, 2 * hh:2 * hh + 2, :], xf[:, 2 * hh:2 * hh + 2, :], [xfB], [xbB])
                if sq_eng == "act":
                    act(xsq[:].rearrange("p c t -> p (c t)"), xf[:].rearrange("p c t -> p (c t)"), AF.Square, [xfB], [xsqB])
                else:
                    for hh in range(2):
                        tt("dve", xsq[:, 4 * hh:4 * hh + 4, :], xf[:, 4 * hh:4 * hh + 4, :], xf[:, 4 * hh:4 * hh + 4, :], ALU.mult,
                           [xfB], [xsqB])
            if "b" not in part:
                return
            bk, bb = nbank()
            for c in range(8):
                mm(bk[:, :], ones_bf, xsq[:, c, :], c == 0, c == 7, [xsqB, P["cB"]], [bb])
            et, etB = P["et"][slot], P["etB"][slot]
            ts("dve", et[:, :], bk[:, :], EPS / 1024.0, ALU.mult, [bb], [etB], s2=EPS * EPS, op1=ALU.add)
            if need_r8:
                r8, r8B = P["r8"][slot], P["r8B"][slot]
                ts("dve", r8[0:8, :], bk[0:8, :], 1.0 / 1024.0, ALU.mult, [bb], [r8B], s2=EPS, op1=ALU.add)
                act(r8[0:8, :], r8[0:8, :], AF.Ln, [r8B], [r8B])
                act(r8[0:8, :], r8[0:8, :], AF.Exp, [r8B], [r8B], scale=-0.5)
            bk2, bb2 = nbank()
            for t4 in range(4):
                for c in range(8):
                    mm(bk2[:, 2 * t4:2 * t4 + 2], xsq[:, c, t4 * 128:(t4 + 1) * 128], cb[:, 0:2], c == 0, c == 7,
                       [xsqB, P["cB"]], [bb2])
            rt, rtB = P["rt"][slot], P["rtB"][slot]
            ts("dve", rt[:, 0:8], bk2[:, 0:8], 1.0 / 1024.0, ALU.mult, [bb2], [rtB], s2=EPS, op1=ALU.add)
            act(rt[:, 0:8], rt[:, 0:8], AF.Ln, [rtB], [rtB])
            act(rt[:, 0:8], rt[:, 0:8], AF.Exp, [rtB], [rtB], scale=-0.5)

        def norm_stages(P, Wt, WB, colofs, xb, xbB, et, etB, gain_ap, rope, out_ap, outB, k, final=None):
            ksq, t, t2, kn = P["nt"][k % 3]
            ksqB, tB, t2B, knB = P["ntB"][k % 3]
            hold = {}

            def s0():
                bk, bb = nbank("raw")
                hold["raw"] = (bk, bb)
                for c in range(8):
                    mm(bk[:, :], Wt[:, c, colofs:colofs + 128], xb[:, c, :], c == 0, c == 7, [WB, xbB], [bb])
                act(ksq[:, :], bk[:, :], AF.Square, [bb], [ksqB])

            def s1():
                bk, bb = hold["raw"]
                b2, bb2 = nbank("h")
                mm(b2[:, :], bdiag, ksq[:, :], True, True, [ksqB, P["cB"]], [bb2])
                stt(t[:, :], b2[:, :], 1.0 / 64.0, et[:, :], ALU.mult, ALU.add, [bb2, etB], [tB])
                act(t[:, :], t[:, :], AF.Ln, [tB], [tB])
                act(t[:, :], t[:, :], AF.Exp, [tB], [tB], scale=-0.5)
                if rope is None:
                    stt(out_ap, bk[:, :], gain_ap, t[:, :], ALU.mult, ALU.mult, [bb, tB, P["cB"]], [outB])
                    if final is not None:
                        final()
                else:
                    stt(kn[:, :], bk[:, :], gain_ap, t[:, :], ALU.mult, ALU.mult, [bb, tB, P["cB"]], [knB])

            def s2():
                cos_ap, sin_ap, csB = rope
                b3, bb3 = nbank("h")
                mm(b3[:, :], RTm, kn[:, :], True, True, [knB, P["cB"]], [bb3])
                tt("pool", t[:, :], kn[:, :], cos_ap, ALU.mult, [knB, csB], [tB])
                tt("dve", t2[:, :], b3[:, :], sin_ap, ALU.mult, [bb3, csB], [t2B])
                tt("pool", out_ap, t[:, :], t2[:, :], ALU.add, [tB, t2B], [outB])
                if final is not None:
                    final()

            return [s0, s1] if rope is None else [s0, s1, s2]

        def alloc_proj(stack, pre, need_r8, nxb=2, nxf=2):
            P = {}
            P["cB"] = constB[0]
            xfl = [sbt(stack, pre + "xf%d" % i, [128, 8, CH], F32) for i in range(nxf)]
            P["xf"] = [xfl[0], xfl[-1]]
            xbl = [sbt(stack, pre + "xb%d" % i, [128, 8, CH], BF16) for i in range(nxb)]
            xsl = [sbt(stack, pre + "xsq%d" % i, [128, 8, CH], BF16) for i in range(nxb)]
            P["xb"] = [xbl[0], xbl[-1]]
            P["xsq"] = [xsl[0], xsl[-1]]
            P["et"] = [sbt(stack, pre + "et%d" % i, [128, CH], F32) for i in range(2)]
            P["rt"] = [sbt(stack, pre + "rt%d" % i, [128, 8], F32) for i in range(2)]
            for nme in ("et", "rt"):
                P[nme + "B"] = [S.buf(pre + nme + "B%d" % i) for i in range(2)]
            xfbl = [S.buf(pre + "xfB%d" % i) for i in range(nxf)]
            P["xfB"] = [xfbl[0], xfbl[-1]]
            P["nxf"] = nxf
            for nme in ("xb", "xsq"):
                bl = [S.buf(pre + nme + "B%d" % i) for i in range(nxb)]
                P[nme + "B"] = [bl[0], bl[-1]]
            if need_r8:
                P["r8"] = [sbt(stack, pre + "r8%d" % i, [8, CH], F32) for i in range(2)]
                P["r8B"] = [S.buf(pre + "r8B%d" % i) for i in range(2)]
            P["nt"] = [(sbt(stack, pre + "ksq%d" % i, [128, CH], BF16), sbt(stack, pre + "t%d" % i, [128, CH], F32),
                        sbt(stack, pre + "t2%d" % i, [128, CH], F32), sbt(stack, pre + "kn%d" % i, [128, CH], BF16))
                       for i in range(3)]
            P["ntB"] = [tuple(S.buf(pre + "ntB%d_%d" % (i, q)) for q in range(4)) for i in range(3)]
            return P

        constB = [None]

        def load_consts():
            constB[0] = S.buf("const")
            cB = constB[0]
            S.dma("sp", cb[:, :], cbf[:, :], (), [cB], chan="c0")
            S.dma("sp", gv[:, :], gvec[:, :], (), [cB], chan="c0")
            S.dma("sp", idf[:, :], identf[:, :], (), [cB], chan="c0")
            S.dma("sp", tab[:, :], tabs[:, :], (), [cB], chan="c0")
            memset("pool", onesf[:, :], 1.0, [cB])

        ucnt = {"n": 0}

        def attention(steps, ptile, ptB, scr, scrB):
            n_steps = len(steps)
            NS = 5
            LA = 3

            def issue_s(n):
                st = steps[n]
                bk, bb = pbank[2 + n % NS], pbuf[2 + n % NS]
                st["sb"] = (bk, bb)
                mm(bk[:, st["qlo"]:512], st["k"], st["q"], True, True, st["kqR"], [bb])

            for n in range(min(LA, n_steps)):
                issue_s(n)
            for n in range(n_steps):
                if n + LA < n_steps:
                    issue_s(n + LA)
                st = steps[n]
                if st["first"]:
                    ucnt["n"] += 1
                u = ucnt["n"] % 2
                ob, obB = pbank[u], pbuf[u]
                bk, bb = st["sb"]
                qlo = st["qlo"]
                pt, pB = ptile[n % 3], ptB[n % 3]
                act(pt[:, qlo:512], bk[:, qlo:512], AF.Exp, [bb] + st["biasR"], [pB], bias=st["bias"], scale=0.125)
                if st["mask"] is not None:
                    st["mask"](pt, pB, qlo)
                for qb in range(qlo // 128, 4):
                    start = bool(st["first"] and qb == 0)
                    last = st["last"](qb)
                    S.op("pe", (lambda o, l, r, a, b: (lambda e: e.matmul(o, l, r, start=a, stop=b, skip_group_check=True)))(
                        ob[:, qb * 65:(qb + 1) * 65], pt[:, qb * 128:(qb + 1) * 128], st["v"], start, last),
                        [pB] + st["vR"], [obB])
                if st["final"]:
                    ti, hglob = st["unit"]
                    ov = ob[:, 0:260].rearrange("p (q e) -> p q e", e=65)
                    rl4 = scr[0][:, 4 * u:4 * u + 4]
                    recip(rl4, ov[:, :, 64], [obB], [scrB])
                    tt("dve", Otok[:, ti * 4:(ti + 1) * 4, hglob * 64:(hglob + 1) * 64], ov[:, :, 0:64],
                       rl4.unsqueeze(2).broadcast_to([128, 4, 64]), ALU.mult, [obB, scrB], [OtokB[0]])

        OtokB = [None]

        new_pbufs()
        load_consts()
        cB = constB[0]
        OtokB[0] = S.buf("Otok")
        with ExitStack() as pw:
            P = alloc_proj(pw, "w", False, nxb=2, nxf=1)
            stg = sbt(pw, "w_stg", [128, 8, 256], F32)
            stgB = S.buf("stgw")
            Ww = sbt(pw, "Ww", [128, 8, 1536], BF16)
            WwB = S.buf("Ww")
            load_weights(pw, cB, "ww", [(1536, 2048), (2048, 2560), (2560, 3072)], stg, stgB, Ww, WwB, g_mix, w_in, "stgw")
            dm = sbt(pw, "dm", [128, 10, 512], BF16)
            S.dma("sp", dm[:].rearrange("p a b -> p (a b)"), dmaskd.rearrange("p a b -> p (a b)"), (), [cB], chan="c0")
            KTw = sbt(pw, "KTw", [128, 4, 5 * CH], BF16)
            KTwB = S.buf("KTw")
            Vw = sbt(pw, "Vw", [128, 20, 8 * 65], BF16)
            VwB = S.buf("Vw")
            memset("pool", Vw[:].rearrange("p a b -> p (a b)"), 1.0, [VwB])
            QTd = sbt(pw, "QTd", [128, 4, CH], BF16)
            QTdB = S.buf("QTd")
            cs = [(sbt(pw, "cos%d" % i, [128, CH], F32), sbt(pw, "sin%d" % i, [128, CH], F32)) for i in range(2)]
            csB = [S.buf("csB%d" % i) for i in range(2)]
            ptile = [sbt(pw, "wpt%d" % i, [128, 512], BF16) for i in range(3)]
            ptB = [S.buf("wptB%d" % i) for i in range(3)]
            scr = (sbt(pw, "wrl", [128, 8], F32), sbt(pw, "wjunk", [128, 64], F32))
            scrB = S.buf("wscr")
            midx = [0, 1, 2, 3] + [4] * 8 + [5, 6, 7, 8, 9] + [9, 9, 9]
            kcount = 0
            def w_pre(g, part):
                ti_, wc_ = g // 5, g % 5
                sl = g % 2
                chunk_pre(P, xTw[ti_, wc_], sl, False, part=part)
                if "a" in part:
                    S.dma("sp", cs[sl][0][:, :], cosw[ti_, :, wc_ * CH:(wc_ + 1) * CH], (), [csB[sl]], chan="cs%d" % sl)
                    S.dma("sp", cs[sl][1][:, :], sinw[ti_, :, wc_ * CH:(wc_ + 1) * CH], (), [csB[sl]], chan="cs%d" % sl)

            w_pre(0, "ab")
            for ti in range(4):
                for wc in range(5):
                    g = ti * 5 + wc
                    slot = g % 2
                    xb, xbB = P["xb"][slot], P["xbB"][slot]
                    et, etB = P["et"][slot], P["etB"][slot]
                    rt, rtB = P["rt"][slot], P["rtB"][slot]
                    rope = (cs[slot][0][:, :], cs[slot][1][:, :], csB[slot])
                    if wc == 0:
                        pipe = Pipe()

                    def vproj(t4, xb=xb, xbB=xbB, rt=rt, rtB=rtB, wc=wc):
                        bk, bb = nbank("m")
                        for c in range(8):
                            mm(bk[:, :], xb[:, c, t4 * 128:(t4 + 1) * 128], Ww[:, c, 1024:1536], c == 0, c == 7, [xbB, WwB], [bb])
                        act(Vw[:, wc * 4 + t4, :].rearrange("p (h e) -> p h e", e=65)[:, :, 0:64],
                            bk[:, :].rearrange("p (h e) -> p h e", e=64), AF.Copy, [bb, rtB], [VwB],
                            scale=rt[:, 2 * t4:2 * t4 + 1])

                    for hp in range(4):
                        if hp == 2 and g + 1 < 20:
                            w_pre(g + 1, "a")
                        pipe.push(norm_stages(P, Ww, WwB, 512 + hp * 128, xb, xbB, et, etB, g_qk(3), rope,
                                              KTw[:, hp, wc * CH:(wc + 1) * CH], KTwB, kcount))
                        kcount += 1
                        vproj(hp)
                    if wc == 4:
                        for hp in range(4):
                            pipe.push(norm_stages(P, Ww, WwB, hp * 128, xb, xbB, et, etB, g_qk(2), rope,
                                                  QTd[:, hp, :], QTdB, kcount))
                            kcount += 1
                    if wc == 4:
                        pipe.drain()
                    if g + 1 < 20:
                        w_pre(g + 1, "b")
                steps = []
                for h in range(8):
                    hp, half = h // 2, h % 2
                    p0 = 64 * half
                    for w in range(20):
                        r = w - 16
                        qlo = max(0, r) * 128
                        mi = midx[w]

                        def mk(pt, pB, qlo, mi=mi, w=w):
                            eng = "dve"
                            tt(eng, pt[:, qlo:512], pt[:, qlo:512], dm[:, mi, 0:512 - qlo], ALU.mult, [pB, cB], [pB])
                        steps.append({
                            "qlo": qlo,
                            "k": KTw[p0:p0 + 64, hp, w * 128:(w + 1) * 128],
                            "q": QTd[p0:p0 + 64, hp, qlo:512],
                            "kqR": [KTwB, QTdB],
                            "bias": deadd(ti, w),
                            "biasR": [cB],
                            "v": Vw[:, w, h * 65:(h + 1) * 65],
                            "vR": [VwB],
                            "first": w == 0,
                            "last": (lambda qb, w=w: w == 16 + qb),
                            "final": w == 19,
                            "unit": (ti, 8 + h),
                            "mask": mk,
                        })
                attention(steps, ptile, ptB, scr, scrB)
            S.flush()

        es_ab = ExitStack()
        QTf = sbt(es_ab, "QTf", [128, 4, 2048], BF16)
        biasT = sbt(es_ab, "biasT", [128, 4, 64, 8], F32)
        new_pbufs()
        load_consts()
        cB = constB[0]
        QTfB = S.buf("QTf")
        biasB = S.buf("biasT")
        KdB = S.buf("Kd")
        VdB = S.buf("Vd")
        with ExitStack() as pa:
            P = alloc_proj(pa, "a", True)
            stg = sbt(pa, "a_stg", [128, 8, 256], F32)
            stgB = S.buf("stg")
            Wa = sbt(pa, "Wa", [128, 8, 1544], BF16)
            WaB = S.buf("Wa")
            load_weights(pa, cB, "wa", [(512, 1024), (1024, 1536), (3072, 3080), (0, 512)], stg, stgB, Wa, WaB,
                         g_mix, w_in, "stg")
            cc = sbt(pa, "cc", [8, 2, CH], F32)
            cTB = S.buf("cT")
            cmid8 = sbt(pa, "cmid8", [8, 16], F32)
            ctok = sbt(pa, "ctok", [128, 64, 8], F32)
            ctokB = S.buf("ctok")
            bf_sb = sbt(pa, "bf_sb", [8, 1], F32)
            oh8 = sbt(pa, "oh8s", [8, 64], F32)
            S.dma("sp", bf_sb[:, :], bfv[:, :], (), [cB], chan="c0")
            S.dma("sp", oh8[:, :], oh8d[:, :], (), [cB], chan="c0")
            ktile = [sbt(pa, "a_kt%d" % i, [128, CH], BF16) for i in range(4)]
            ktB = [S.buf("a_ktB%d" % i) for i in range(4)]
            vsb = [sbt(pa, "a_v%d" % i, [128, 4, 4, 130], BF16) for i in range(2)]
            vsbB = [S.buf("a_vB%d" % i) for i in range(2)]
            for i in range(2):
                memset("pool", vsb[i][:].rearrange("p a b c -> p (a b c)"), 1.0, [vsbB[i]])
            fz = sbt(pa, "a_fz", [8, 3, CH], F32)
            fzB = S.buf("fz")
            kcount = 0
            deferred = []
            def a_pre(kc_, part):
                if kc_ < NCH:
                    chunk_pre(P, xT[kc_], kc_ % 2, True, part=part)
                else:
                    chunk_pre(P, xTw[kc_ - NCH, 4], kc_ % 2, False, part=part)

            a_pre(0, "ab")
            for kc in range(NCH + 4):
                slot = kc % 2
                is_q = kc >= NCH
                if is_q:
                    while deferred:
                        deferred.pop(0)()
                xb, xbB = P["xb"][slot], P["xbB"][slot]
                et, etB = P["et"][slot], P["etB"][slot]
                rt, rtB = P["rt"][slot], P["rtB"][slot]
                if kc == 0:
                    pipe = Pipe()
                if is_q:
                    ti = kc - NCH
                    for hp in range(4):
                        if hp == 2 and kc + 1 < NCH + 4:
                            a_pre(kc + 1, "a")
                        pipe.push(norm_stages(P, Wa, WaB, 1032 + hp * 128, xb, xbB, et, etB, g_qk(0), None,
                                              QTf[:, hp, ti * CH:(ti + 1) * CH], QTfB, kcount))
                        kcount += 1
                    if kc + 1 < NCH + 4:
                        a_pre(kc + 1, "b")
                    else:
                        pipe.drain()
                    continue
                vs = kc % 2

                def vproj(t4, xb=xb, xbB=xbB, rt=rt, rtB=rtB, vs=vs):
                    bk, bb = nbank("m")
                    for c in range(8):
                        mm(bk[:, :], xb[:, c, t4 * 128:(t4 + 1) * 128], Wa[:, c, 512:1024], c == 0, c == 7, [xbB, WaB], [bb])
                    act(vsb[vs][:, :, t4, :].rearrange("p a (h e) -> p a h e", e=65)[:, :, :, 0:64],
                        bk[:, :].rearrange("p (a h e) -> p a h e", a=4, e=64), AF.Copy, [bb, rtB], [vsbB[vs]],
                        scale=rt[:, 2 * t4:2 * t4 + 1])

                for hp in range(4):
                    if hp == 2:
                        while deferred:
                            deferred.pop(0)()
                        if kc + 1 < NCH + 4:
                            a_pre(kc + 1, "a")
                    ks = kcount % 4

                    def spill(hp=hp, ks=ks, kc=kc):
                        S.dma("pool", Kd[hp, :, kc * CH:(kc + 1) * CH], ktile[ks][:, :], [ktB[ks]], [KdB], chan="ks%d" % ks)
                    pipe.push(norm_stages(P, Wa, WaB, hp * 128, xb, xbB, et, etB, g_qk(1), None,
                                          ktile[ks][:, :], ktB[ks], kcount, final=spill))
                    kcount += 1
                    vproj(hp)
                for hp in range(4):
                    S.dma("pool", Vd[hp, :, kc * 4:(kc + 1) * 4, :], vsb[vs][:, hp, :, :],
                          [vsbB[vs]], [VdB], chan="vs%d" % vs)
                bk, bb = nbank()
                for c in range(8):
                    mm(bk[0:8, :], Wa[:, c, 1024:1032], xb[:, c, :], c == 0, c == 7, [WaB, xbB], [bb])
                r8, r8B = P["r8"][slot], P["r8B"][slot]
                z, mneg, na = fz[:, 0, :], fz[:, 1, :], fz[:, 2, :]
                tt("dve", z, bk[0:8, :], r8[0:8, :], ALU.mult, [bb, r8B], [fzB])
                ts("dve", z, z, bf_sb[:, 0:1], ALU.add, [fzB, cB], [fzB])
                ts("dve", mneg, z, 0.0, ALU.min, [fzB], [fzB])
                ts("dve", na, z, -1.0, ALU.mult, [fzB], [fzB])
                tt("dve", na, na, z, ALU.min, [fzB], [fzB])
                act(na, na, AF.Exp, [fzB], [fzB])
                act(na, na, AF.Ln, [fzB], [fzB], bias=1.0)
                tt("dve", z, mneg, na, ALU.subtract, [fzB], [fzB])
                init = 0.0 if kc == 0 else cc[0:8, (kc - 1) % 2, CH - 1:CH]
                S.op("dve", (lambda o, d0, d1, ini: (lambda e: e.tensor_tensor_scan(out=o, data0=d0, data1=d1, initial=ini,
                                                                                     op0=ALU.mult, op1=ALU.add)))(
                    cc[0:8, kc % 2, :], onesf[0:8, 0:1].broadcast_to([8, CH]), z, init), [fzB, cTB, cB], [cTB])
                cp("dve", cmid8[:, kc:kc + 1], cc[0:8, kc % 2, 255:256], [cTB], [cTB])

                def ctok_tr(kc=kc):
                    bk, bb = nbank()
                    for t4 in range(4):
                        tr(bk[:, 8 * t4:8 * t4 + 8], cc[0:8, kc % 2, t4 * 128:(t4 + 1) * 128], idf[0:8, 0:8], [cTB, cB], [bb])
                    cp("dve", ctok[:, kc * 4:(kc + 1) * 4, :].rearrange("p a b -> p (a b)"), bk[:, 0:32], [bb], [ctokB])
                deferred.append(ctok_tr)
                if kc + 1 < NCH + 4:
                    a_pre(kc + 1, "b")
            crw = sbt(pa, "crw", [8, 64], F32)
            crB = S.buf("cr")
            cmid = cmid8[:, :].unsqueeze(1).broadcast_to([8, 4, 16])
            tt("dve", crw[:, :].rearrange("p (a b) -> p a b", b=16), oh8[:, :].rearrange("p (a b) -> p a b", b=16),
               cmid, ALU.mult, [cTB, cB], [crB])
            cr4 = sbt(pa, "cr4", [8, 4], F32)
            S.op("dve", (lambda o, i_: (lambda e: e.tensor_reduce(out=o, in_=i_, axis=AX.X, op=ALU.add)))(
                cr4[:, :], crw[:, :].rearrange("p (a b) -> p a b", b=16)), [crB], [crB])
            rd = sbt(pa, "rd", [8, 4, 8], F32)
            for ti in range(4):
                ts("dve", rd[:, ti, :], idf[0:8, 0:8], cr4[:, ti:ti + 1], ALU.mult, [crB, cB], [crB])
            bk, bb = nbank()
            mm(bk[:, 0:32], onesf[0:8, :], rd[:].rearrange("p a b -> p (a b)"), True, True, [crB, cB], [bb])
            crb = sbt(pa, "crb", [128, 32], F32)
            cp("dve", crb[:, :], bk[:, 0:32], [bb], [crB])
            for ti in range(4):
                crb_b = crb[:, ti * 8:(ti + 1) * 8].unsqueeze(1).broadcast_to([128, 64, 8])
                tt("dve", biasT[:, ti, :, :], crb_b, ctok[:, :, :], ALU.subtract, [crB, ctokB], [biasB])
                dead_b = tab[:, ti * 64:(ti + 1) * 64].unsqueeze(2).broadcast_to([128, 64, 8])
                tt("dve", biasT[:, ti, :, :], biasT[:, ti, :, :], dead_b, ALU.add, [biasB, cB], [biasB])
            S.flush()

        new_pbufs()
        load_consts()
        cB = constB[0]
        OtokB[0] = S.buf("Otok")
        QTfB = S.buf("QTf")
        biasB = S.buf("biasT")
        with ExitStack() as pbk:
            KTp = [sbt(pbk, "KTp%d" % i, [128, S_LEN], BF16) for i in range(2)]
            V2 = [sbt(pbk, "V2%d" % i, [128, 64, 130], BF16) for i in range(2)]
            KTpB = [S.buf("KTpB%d" % i) for i in range(2)]
            V2B = [S.buf("V2B%d" % i) for i in range(2)]
            fm = sbt(pbk, "fm", [128, 4, 512], BF16)
            S.dma("sp", fm[:].rearrange("p a b -> p (a b)"), fmaskd.rearrange("p a b -> p (a b)"), (), [cB], chan="c0")
            ptile = [sbt(pbk, "pt%d" % i, [128, 512], BF16) for i in range(3)]
            ptB = [S.buf("ptB%d" % i) for i in range(3)]
            scr = (sbt(pbk, "rl", [128, 8], F32), sbt(pbk, "junk", [128, 64], F32))
            scrB = S.buf("scr")

            def load_pair(hp):
                s_ = hp % 2
                for q4 in range(4):
                    S.dma("sp", KTp[s_][:, q4 * 2048:(q4 + 1) * 2048], Kd[hp, :, q4 * 2048:(q4 + 1) * 2048], (), [KTpB[s_]],
                          chan="kl%d" % s_)
                    S.dma("sp", V2[s_][:, q4 * 16:(q4 + 1) * 16, :], Vd[hp, :, q4 * 16:(q4 + 1) * 16, :], (), [V2B[s_]],
                          chan="vl%d" % s_)

            steps = []
            load_pair(0)
            for hp in range(4):
                s_ = hp % 2
                for half in range(2):
                    h = hp * 2 + half
                    p0 = 64 * half
                    for ti in range(4):
                        nkt = 16 * (ti + 1)
                        for kt in range(nkt):
                            kcr = kt // 4 - 4 * ti
                            d = kt % 4
                            st = {
                                "qlo": 0,
                                "k": KTp[s_][p0:p0 + 64, kt * 128:(kt + 1) * 128],
                                "q": QTf[p0:p0 + 64, hp, ti * CH:(ti + 1) * CH],
                                "kqR": [KTpB[s_], QTfB],
                                "bias": biasT[:, ti, kt, h:h + 1],
                                "biasR": [biasB],
                                "v": V2[s_][:, kt, half * 65:(half + 1) * 65],
                                "vR": [V2B[s_]],
                                "first": kt == 0,
                                "last": (lambda qb, kt=kt, nkt=nkt: kt == nkt - 1),
                                "final": kt == nkt - 1,
                                "unit": (ti, h),
                                "mask": None,
                            }
                            if kcr >= 0:
                                def mk(pt, pB, qlo, d=d, ti=ti, kcr=kcr):
                                    stt(pt[:, :], fm[:, d, :], amask(ti, kcr), pt[:, :], ALU.max, ALU.mult, [pB, cB], [pB])
                                st["mask"] = mk
                            steps.append(st)
            per_pair = len(steps) // 4
            for hp in range(4):
                if hp + 1 < 4:
                    load_pair(hp + 1)
                attention(steps[hp * per_pair:(hp + 1) * per_pair], ptile, ptB, scr, scrB)
            S.flush()
        es_ab.close()

        new_pbufs()
        load_consts()
        cB = constB[0]
        OtokB[0] = S.buf("Otok")
        with ExitStack() as pc:
            hsb = sbt(pc, "hsb", [128, 16, D], F32)
            hB = [S.buf("hB%d" % i) for i in range(16)]
            hnT = sbt(pc, "hnT", [128, 8, 2048], BF16)
            hnTB = S.buf("hnT")
            comb = sbt(pc, "comb", [128, 16, NE], F32)
            combB = S.buf("comb")
            br_sb = sbt(pc, "br_sb", [128, 36], F32)
            S.dma("sp", br_sb[:, :], brv[:, :], (), [cB], chan="c0")
            with ExitStack() as pcc:
                stg = sbt(pcc, "c_stg", [128, 8, 256], F32)
                stgB = S.buf("stgc")
                Wo = sbt(pcc, "Wo", [128, 8, D], BF16)
                WoB = S.buf("Wo")
                load_weights(pcc, cB, "wo", [(0, 1024)], stg, stgB, Wo, WoB, g_out, w_out, "stgc")
                wrs = sbt(pcc, "wrs", [128, 8, 36], F32)
                S.dma("sp", wrs[:, :, :], wr[:, :, :], (), [cB], chan="c0")
                ro = sbt(pcc, "ro", [128, 32], F32)
                roB = S.buf("ro")
                rjunk = sbt(pcc, "rjunk", [128, 512], BF16)
                rjB = S.buf("rjunk")
                for i in range(16):
                    for g in range(2):
                        act(rjunk[:, :], Otok[:, i, g * 512:(g + 1) * 512], AF.Square, [OtokB[0]], [rjB, roB],
                            accum_out=ro[:, 2 * i + g:2 * i + g + 1])
                ts("dve", ro[:, :], ro[:, :], 1.0 / 512.0, ALU.mult, [roB], [roB], s2=EPS, op1=ALU.add)
                act(ro[:, :], ro[:, :], AF.Ln, [roB], [roB])
                act(ro[:, :], ro[:, :], AF.Exp, [roB], [roB], scale=-0.5)
                for i in range(16):
                    for g in range(2):
                        act(Otok[:, i, g * 512:(g + 1) * 512], Otok[:, i, g * 512:(g + 1) * 512], AF.Copy,
                            [OtokB[0], roB], [OtokB[0]], scale=ro[:, 2 * i + g:2 * i + g + 1])
                oT = [sbt(pcc, "oT%d" % i, [128, 8, 128], BF16) for i in range(2)]
                oTB = [S.buf("oTB%d" % i) for i in range(2)]
                xo = [sbt(pcc, "xo%d" % i, [128, D], F32) for i in range(2)]
                xoB = [S.buf("xoB%d" % i) for i in range(2)]
                hs = [sbt(pcc, "hs%d" % i, [128, D], F32) for i in range(2)]
                hsB_ = [S.buf("hsB%d" % i) for i in range(2)]
                hTf = sbt(pcc, "hTf0", [128, 8, 128], F32)
                hTfB = S.buf("hTfB0")
                rh = sbt(pcc, "rh", [128, 16], F32)
                rhB = [S.buf("rh%d" % i) for i in range(16)]
                lg = sbt(pcc, "lg", [128, 16, 36], F32)
                lgBs = [S.buf("lg%d" % i) for i in range(16)]
                pbbB = S.buf("pbb")

                def c_stages(i):
                    s_ = i % 2

                    def s0():
                        S.dma("sp", xo[s_][:, :], xown[i * 128:(i + 1) * 128, :], (), [xoB[s_]], chan="xo%d" % s_)
                        for c in range(8):
                            tr(pbb[:, c * 128:(c + 1) * 128], Otok[:, i, c * 128:(c + 1) * 128], idb, [OtokB[0], cB], [pbbB])
                        cp("dve", oT[s_][:].rearrange("p a b -> p (a b)"), pbb[:, :], [pbbB], [oTB[s_]])

                    def s1():
                        for hf in range(2):
                            bk, bb = nbank("c1")
                            for c in range(8):
                                mm(bk[:, :], oT[s_][:, c, :], Wo[:, c, hf * 512:(hf + 1) * 512], c == 0, c == 7, [oTB[s_], WoB], [bb])
                            tt("dve", hsb[:, i, hf * 512:(hf + 1) * 512], bk[:, :], xo[s_][:, hf * 512:(hf + 1) * 512], ALU.add,
                               [bb, xoB[s_]], [hB[i]])
                        act(hs[s_][:, :], hsb[:, i, :], AF.Square, [hB[i]], [hsB_[s_], rhB[i]], accum_out=rh[:, i:i + 1])
                        ts("dve", rh[:, i:i + 1], rh[:, i:i + 1], 1.0 / 1024.0, ALU.mult, [rhB[i]], [rhB[i]], s2=EPS, op1=ALU.add)
                        act(rh[:, i:i + 1], rh[:, i:i + 1], AF.Ln, [rhB[i]], [rhB[i]])
                        act(rh[:, i:i + 1], rh[:, i:i + 1], AF.Exp, [rhB[i]], [rhB[i]], scale=-0.5)
                        act(hs[s_][:, :], hsb[:, i, :], AF.Copy, [hB[i], rhB[i]], [hsB_[s_]], scale=rh[:, i:i + 1])

                    def s2():
                        for half8 in range(2):
                            bk, bb = nbank("c2")
                            for c4 in range(4):
                                c = half8 * 4 + c4
                                tr(bk[:, c4 * 128:(c4 + 1) * 128], hs[s_][:, c * 128:(c + 1) * 128], idf[:, :], [hsB_[s_], cB], [bb])
                            for c4 in range(4):
                                c = half8 * 4 + c4
                                ts("dve", hTf[:, c, :], bk[:, c4 * 128:(c4 + 1) * 128], g_ffn(c), ALU.mult, [bb, cB], [hTfB])
                        cp("dve", hnT[:, :, i * 128:(i + 1) * 128], hTf[:, :, :], [hTfB], [hnTB])

                    def s3():
                        bk, bb = nbank("r")
                        for c in range(8):
                            mm(bk[:, 0:36], hTf[:, c, :], wrs[:, c, :], c == 0, c == 7, [hTfB, cB], [bb])
                        tt("dve", lg[:, i, :], bk[:, 0:36], br_sb[:, :], ALU.add, [bb, cB], [lgBs[i]])
                    return [s0, s1, s2, s3]

                pipe = Pipe()
                for i in range(16):
                    pipe.push(c_stages(i))
                pipe.drain()
                lgB = S.buf("lgall")
                S.op("dve", (lambda a: (lambda e: e.tensor_copy(out=a, in_=a)))(lg[:, 0, 0:1]), lgBs, [lgB])
                rw = sbt(pcc, "rw", [128, 16, 64], F32)
                rwB = S.buf("rw")
                zg = lg[:, :, 0:4]
                ze = lg[:, :, 4:36]
                gmax = rw[:, :, 0:1]
                S.op("dve", (lambda o, i_: (lambda e: e.tensor_reduce(out=o, in_=i_, axis=AX.X, op=ALU.max)))(
                    rw[:, :, 0], zg), [lgB], [rwB])
                goh = rw[:, :, 4:8]
                tt("dve", goh, zg, gmax.broadcast_to([128, 16, 4]), ALU.is_equal, [lgB, rwB], [rwB])
                gex = rw[:, :, 8:12]
                tt("dve", gex, zg, gmax.broadcast_to([128, 16, 4]), ALU.subtract, [lgB, rwB], [rwB])
                act(gex, gex, AF.Exp, [rwB], [rwB])
                S.op("dve", (lambda o, i_: (lambda e: e.tensor_reduce(out=o, in_=i_, axis=AX.X, op=ALU.add)))(
                    rw[:, :, 1], gex), [rwB], [rwB])
                pgt = rw[:, :, 1:2]
                recip(pgt, pgt, [rwB], [rwB])
                zs = rw[:, :, 16:24]
                ztmp = rw[:, :, 24:32]
                for g in range(4):
                    gsel = rw[:, :, 4 + g:5 + g].broadcast_to([128, 16, 8])
                    if g == 0:
                        tt("dve", zs, ze[:, :, 0:8], gsel, ALU.mult, [lgB, rwB], [rwB])
                    else:
                        tt("dve", ztmp, ze[:, :, g * 8:(g + 1) * 8], gsel, ALU.mult, [lgB, rwB], [rwB])
                        tt("dve", zs, zs, ztmp, ALU.add, [rwB], [rwB])
                m1 = rw[:, :, 2:3]
                S.op("dve", (lambda o, i_: (lambda e: e.tensor_reduce(out=o, in_=i_, axis=AX.X, op=ALU.max)))(
                    rw[:, :, 2], zs), [rwB], [rwB])
                oh1 = rw[:, :, 32:40]
                tt("dve", oh1, zs, m1.broadcast_to([128, 16, 8]), ALU.is_equal, [rwB], [rwB])
                zm = rw[:, :, 40:48]
                S.op("dve", (lambda o, a, b: (lambda e: e.scalar_tensor_tensor(out=o, in0=a, scalar=-1e30, in1=b, op0=ALU.mult,
                                                                                 op1=ALU.add)))(zm, oh1, zs), [rwB], [rwB])
                m2 = rw[:, :, 3:4]
                S.op("dve", (lambda o, i_: (lambda e: e.tensor_reduce(out=o, in_=i_, axis=AX.X, op=ALU.max)))(
                    rw[:, :, 3], zm), [rwB], [rwB])
                oh2 = rw[:, :, 48:56]
                tt("dve", oh2, zm, m2.broadcast_to([128, 16, 8]), ALU.is_equal, [rwB], [rwB])
                ee = rw[:, :, 12:13]
                tt("dve", ee, m2, m1, ALU.subtract, [rwB], [rwB])
                act(ee, ee, AF.Exp, [rwB], [rwB])
                den = rw[:, :, 13:14]
                ts("dve", den, ee, 1.0, ALU.add, [rwB], [rwB])
                recip(den, den, [rwB], [rwB])
                g1 = rw[:, :, 14:15]
                tt("dve", g1, den, pgt, ALU.mult, [rwB], [rwB])
                g2 = rw[:, :, 15:16]
                tt("dve", g2, g1, ee, ALU.mult, [rwB], [rwB])
                we = rw[:, :, 56:64]
                tt("dve", oh1, oh1, g1.broadcast_to([128, 16, 8]), ALU.mult, [rwB], [rwB])
                tt("dve", oh2, oh2, g2.broadcast_to([128, 16, 8]), ALU.mult, [rwB], [rwB])
                tt("dve", we, oh1, oh2, ALU.add, [rwB], [rwB])
                for g in range(4):
                    gsel = rw[:, :, 4 + g:5 + g].broadcast_to([128, 16, 8])
                    tt("dve", comb[:, :, g * 8:(g + 1) * 8], we, gsel, ALU.mult, [rwB], [combB])
                S.flush()

            new_pbufs()
            load_consts()
            cB = constB[0]
            hB = [S.buf("hB%d" % i) for i in range(16)]
            hnTB = S.buf("hnT")
            combB = S.buf("comb")
            with ExitStack() as pd:
                NST = 6
                stg4 = [sbt(pd, "d_stg%d" % i, [128, 1024], F32) for i in range(NST)]
                stg4B = [S.buf("d_stgB%d" % i) for i in range(NST)]
                wgb = [oreg[:, i * 4096:(i + 1) * 4096].rearrange("p (c n) -> p c n", n=512) for i in range(2)]
                wub = [oreg[:, 8192 + i * 4096:8192 + (i + 1) * 4096].rearrange("p (c n) -> p c n", n=512) for i in range(2)]
                wdb = [sbt(pd, "wdb%d" % i, [128, 4, D], BF16) for i in range(2)]
                wB = [[S.buf("wB%d_%d" % (k, i)) for i in range(2)] for k in range(3)]
                gs = [sbt(pd, "gs%d" % i, [128, 512], BF16) for i in range(2)]
                gsB = [S.buf("gsB%d" % i) for i in range(2)]
                HT = [sbt(pd, "HT%d" % i, [128, 4, 512], BF16) for i in range(2)]
                HTB = [S.buf("HTB%d" % i) for i in range(2)]
                sc = {"i": 0}

                def load_expert(e):
                    s_ = e % 2
                    pieces = []
                    for k, (src, dst) in enumerate(((wg, wgb), (wu, wub))):
                        v = src[e]
                        for q in range(4):
                            pieces.append((v[:, 2 * q:2 * q + 2, :], dst[s_][:, 2 * q:2 * q + 2, :], wB[k][s_], 2, 512))
                    v = wd[e]
                    for q in range(4):
                        pieces.append((v[:, q:q + 1, :], wdb[s_][:, q:q + 1, :], wB[2][s_], 1, 1024))
                    todo = []
                    for (sv, dv, db, a, b) in pieces:
                        def one(sv=sv, dv=dv, db=db, a=a):
                            i = sc["i"] % NST
                            sc["i"] += 1
                            S.dma("sp", stg4[i][:, :].rearrange("p (a b) -> p a b", a=a), sv, (), [stg4B[i]], chan="ds%d" % i)
                            cp("pool", dv, stg4[i][:, :].rearrange("p (a b) -> p a b", a=a), [stg4B[i]], [db])
                        todo.append(one)
                    return todo

                for one in load_expert(0):
                    one()
                ucount = 0
                for e in range(NE):
                    s_ = e % 2
                    todo = load_expert(e + 1) if e + 1 < NE else []
                    for tq in range(4):
                        hs_ = (e * 4 + tq) % 2
                        for fc in range(4):
                            if todo:
                                todo.pop(0)()
                            bg, bgB = nbank("all")
                            for c in range(8):
                                mm(bg[:, :], wgb[s_][:, c, fc * 128:(fc + 1) * 128], hnT[:, c, tq * 512:(tq + 1) * 512], c == 0, c == 7,
                                   [wB[0][s_], hnTB], [bgB])
                            bu, buB = nbank("all")
                            for c in range(8):
                                mm(bu[:, :], wub[s_][:, c, fc * 128:(fc + 1) * 128], hnT[:, c, tq * 512:(tq + 1) * 512], c == 0, c == 7,
                                   [wB[1][s_], hnTB], [buB])
                            g_ = ucount % 2
                            ucount += 1
                            act(gs[g_][:, :], bg[:, :], AF.Silu, [bgB], [gsB[g_]])
                            tt("dve", HT[hs_][:, fc, :], bu[:, :], gs[g_][:, :], ALU.mult, [buB, gsB[g_]], [HTB[hs_]])
                        for t4 in range(4):
                            i = tq * 4 + t4
                            for hf in range(2):
                                by, byB = nbank("all")
                                for fc in range(4):
                                    mm(by[:, :], HT[hs_][:, fc, t4 * 128:(t4 + 1) * 128], wdb[s_][:, fc, hf * 512:(hf + 1) * 512],
                                       fc == 0, fc == 3, [HTB[hs_], wB[2][s_]], [byB])
                                stt(hsb[:, i, hf * 512:(hf + 1) * 512], by[:, :], comb[:, i, e:e + 1], hsb[:, i, hf * 512:(hf + 1) * 512],
                                    ALU.mult, ALU.add, [byB, combB, hB[i]], [hB[i]])
                yB = S.buf("y")
                for i in range(16):
                    S.dma("sp", y[i * 128:(i + 1) * 128, :], hsb[:, i, :], [hB[i]], [yB], chan="yo%d" % (i % 2))
                S.flush()
        es_bc.close()
        print("ops per engine:", S.total, flush=True)
    return nc


def _consts():
    ones = np.ones((128, 128), np.float32)
    bd = np.zeros((128, 128), np.float32)
    bd[:64, :64] = 1.0
    bd[64:, 64:] = 1.0
    RT = np.zeros((128, 128), np.float32)
    for i in range(128):
        if i % 64 < 32:
            RT[i + 32, i] = -1.0
        else:
            RT[i - 32, i] = 1.0
    ident = np.eye(128, dtype=np.float32)
    cbf = np.concatenate([ones, bd, RT, ident], axis=1).astype(NBF)
    k = np.arange(128)[:, None]
    q = np.arange(512)[None, :]
    fmask = np.stack([(q >= 128 * d + k) for d in range(4)], axis=1).astype(np.float32).astype(NBF)

    def count(delta):
        c = ((delta >= 0) & (delta <= 128)).astype(np.float32)
        c += ((delta >= 0) & (delta <= 512) & (delta % 4 == 0)).astype(np.float32)
        c += ((delta >= 0) & (delta <= 2048) & (delta % 16 == 0)).astype(np.float32)
        return c
    rs = [-16, -15, -14, -13, -12, -4, -3, -2, -1, 0]
    dmask = np.stack([count(q - k - 128 * r) for r in rs], axis=1).astype(NBF)
    for r in range(-12, -4):
        assert np.array_equal(count(q - k - 128 * r), count(q - k + 128 * 12))
    return cbf, fmask, dmask, ident


def _rope_tables(pos):
    inv_freq = (1.0 / (np.float32(10000.0) ** (np.arange(0, 64, 2, dtype=np.float32) / np.float32(64)))).astype(np.float32)
    ang = (pos.astype(np.float32)[:, None] * inv_freq[None, :]).astype(np.float32)
    cos = np.cos(ang).astype(np.float32).T
    sin = np.sin(ang).astype(np.float32).T
    cosT = np.concatenate([cos, cos, cos, cos], axis=0)
    sinT = np.concatenate([sin, sin, sin, sin], axis=0)
    return np.ascontiguousarray(cosT), np.ascontiguousarray(sinT)


_NC_CACHE = {}


def kernel(x, norm_mix, w_in, b_forget, q_norm_fox, k_norm_fox, q_norm_dil, k_norm_dil,
           out_norm_fox, out_norm_dil, w_out, norm_ffn, w_router_group, b_router_group,
           w_router_expert, b_router_expert, w_gate, w_up, w_down):
    f = lambda a: np.ascontiguousarray(np.asarray(a, dtype=np.float32))
    x = f(x)
    cbf, fmask, dmask, ident = _consts()
    pc = lambda v: np.ascontiguousarray(f(v).reshape(8, 128).T)
    t2 = lambda v: np.tile(f(v).reshape(64), 2).reshape(128, 1)
    gvec = np.concatenate([pc(norm_mix[0]), pc(norm_ffn[0]),
                           pc(np.concatenate([f(out_norm_fox[0]), f(out_norm_dil[0])])),
                           t2(q_norm_fox[0]), t2(k_norm_fox[0]), t2(q_norm_dil[0]), t2(k_norm_dil[0])], axis=1)
    gvec = np.ascontiguousarray(gvec.astype(np.float32))
    bfv = f(b_forget[0]).reshape(8, 1)
    wr = np.ascontiguousarray(np.concatenate(
        [f(w_router_group[0]), f(w_router_expert[0]).transpose(1, 0, 2).reshape(D, 32)], axis=1))
    brv = np.ascontiguousarray(np.broadcast_to(
        np.concatenate([f(b_router_group[0]).reshape(4), f(b_router_expert[0]).reshape(32)])[None, :], (128, 36)))
    pm = lambda a, c: np.ascontiguousarray(a.reshape(a.shape[:-2] + (c, 128, a.shape[-1])).swapaxes(-3, -2))
    shared = {
        "w_in": pm(f(w_in[0]), 8), "w_out": pm(f(w_out[0]), 8), "wg": pm(f(w_gate[0]), 8), "wu": pm(f(w_up[0]), 8),
        "wd": pm(f(w_down[0]), 4), "wr": pm(wr, 8), "gvec": gvec, "bfv": bfv, "brv": brv, "cbf": cbf, "fmask": fmask, "dmask": dmask, "identf": ident,
    }
    xTs = [np.ascontiguousarray(x[b].T) for b in range(2)]
    cpm = lambda xt: np.ascontiguousarray(xt.reshape(8, 128, -1, CH).transpose(2, 1, 0, 3))
    xTc = [cpm(xTs[b]) for b in range(2)]
    in_maps = []
    for c in range(8):
        b, j = c // 4, c % 4
        own = own_chunks(j)
        xTw = np.zeros((4, D, 5 * CH), np.float32)
        cosw = np.zeros((4, 128, 5 * CH), np.float32)
        sinw = np.zeros((4, 128, 5 * CH), np.float32)
        tabs = np.zeros((128, 352), np.float32)
        oh8 = np.zeros((8, 4, 16), np.float32)
        for ti, ch in enumerate(own):
            lo = (ch - 4) * CH
            pos = np.arange(lo, lo + 5 * CH)
            valid = pos >= 0
            xTw[ti][:, valid] = xTs[b][:, pos[valid]]
            cw, sw = _rope_tables(np.maximum(pos, 0).astype(np.float32))
            cosw[ti], sinw[ti] = cw, sw
            for kt in range(64):
                tabs[:, ti * 64 + kt] = 0.0 if (kt // 4) <= ch else NEGB
            for kr in range(4):
                tabs[:, 256 + ti * 4 + kr] = 0.0 if (4 * ti + kr) == ch else 1.0
            for w in range(20):
                tabs[:, 272 + ti * 20 + w] = 0.0 if (lo + w * 128) >= 0 else NEGB
            oh8[:, ti, ch] = 1.0
        xown = np.concatenate([x[b, ch * CH:(ch + 1) * CH, :] for ch in own], axis=0)
        m = dict(shared)
        m.update({"xT": xTc[b], "xTw": np.stack([cpm(xTw[t_]) for t_ in range(4)]), "xown": np.ascontiguousarray(xown), "cosw": cosw, "sinw": sinw,
                  "tabs": tabs, "oh8": np.ascontiguousarray(oh8.reshape(8, 64))})
        in_maps.append(m)
    if "nc" not in _NC_CACHE:
        _NC_CACHE["nc"] = build()
    nc = _NC_CACHE["nc"]
    res = run_bass_kernel_spmd(nc, in_maps, core_ids=list(range(8)))
    out = np.zeros((2, S_LEN, D), np.float32)
    for c in range(8):
        b, j = c // 4, c % 4
        yc = np.asarray(res.results[c]["y"], dtype=np.float32)
        for ti, ch in enumerate(own_chunks(j)):
            out[b, ch * CH:(ch + 1) * CH, :] = yc[ti * CH:(ti + 1) * CH, :]
    return out
```

```python
import numpy as np
import ml_dtypes
from contextlib import ExitStack
import concourse.bass as bass
import concourse.mybir as mybir
from concourse.bass_utils import run_bass_kernel_spmd

F32 = mybir.dt.float32
BF16 = mybir.dt.bfloat16
AF = mybir.ActivationFunctionType
ALU = mybir.AluOpType
AX = mybir.AxisListType

D = 1024
S_LEN = 8192
CH = 512
NCH = 16
EPS = 1e-6
NEGB = -30000.0
NE = 32
NBF = ml_dtypes.bfloat16


def own_chunks(j):
    return [j, 7 - j, 8 + j, 15 - j]


class Buf:
    __slots__ = ("name", "w", "r")

    def __init__(self, name):
        self.name = name
        self.w = {}
        self.r = {}


class Op:
    __slots__ = ("eng", "fn", "deps", "chan", "signal", "value", "idx")


class Sched:
    ENGS = ("pe", "act", "dve", "pool", "sp")

    def __init__(self, nc, es):
        self.nc = nc
        self.es = es
        self.ops = []
        self.bufs = []
        self.cnt = {e: 0 for e in self.ENGS}
        self.ccnt = {}
        self.sems = {e: es.enter_context(nc.semaphore("s_" + e)) for e in self.ENGS}
        self.chan_sems = {}
        self.total = {e: 0 for e in self.ENGS}

    def buf(self, name="b"):
        b = Buf(name)
        self.bufs.append(b)
        return b

    def _add(self, eng, fn, reads, writes, chan=None):
        op = Op()
        op.eng = eng
        op.fn = fn
        op.chan = chan
        op.signal = chan is not None
        op.value = None
        op.idx = len(self.ops)
        key = ("c", chan) if chan is not None else ("e", eng)
        deps = set()
        for b in reads:
            deps.update(b.w.values())
        for b in writes:
            deps.update(b.w.values())
            deps.update(b.r.values())
        if eng == "pe" and chan is None:
            deps = {i for i in deps if not (self.ops[i].eng == "pe" and self.ops[i].chan is None)}
        op.deps = deps
        for i in deps:
            self.ops[i].signal = True
        for b in reads:
            b.r[key] = op.idx
        for b in writes:
            b.w[key] = op.idx
        self.ops.append(op)
        if chan is not None and chan not in self.chan_sems:
            self.chan_sems[chan] = self.es.enter_context(self.nc.semaphore("c_%s" % chan))
        return op

    def op(self, eng, fn, reads=(), writes=()):
        return self._add(eng, fn, reads, writes)

    def dma(self, queue, out, in_, reads=(), writes=(), chan=None):
        return self._add(queue, lambda e: e.dma_start(out=out, in_=in_), reads, writes, chan=chan)

    def flush(self):
        nc = self.nc
        ops = self.ops
        for op in ops:
            if op.chan is not None:
                self.ccnt[op.chan] = self.ccnt.get(op.chan, 0) + 16
                op.value = self.ccnt[op.chan]
            elif op.signal:
                self.cnt[op.eng] += 1
                op.value = self.cnt[op.eng]
        for e, c in self.cnt.items():
            assert c < 60000, (e, c)
        per_eng = {e: [] for e in self.ENGS}
        for op in ops:
            per_eng[op.eng].append(op)
            self.total[op.eng] += 1
        sems, chan_sems = self.sems, self.chan_sems

        def run_engine(ename, h):
            waited = {}
            for op in per_eng[ename]:
                need = {}
                for i in op.deps:
                    d = ops[i]
                    if d.chan is not None:
                        sem, k = chan_sems[d.chan], ("c", d.chan)
                    else:
                        sem, k = sems[d.eng], ("e", d.eng)
                    if need.get(k, (None, 0))[1] < d.value:
                        need[k] = (sem, d.value)
                for k, (sem, v) in need.items():
                    if waited.get(k, 0) < v:
                        h.wait_ge(sem, v)
                        waited[k] = v
                inst = op.fn(h)
                if op.chan is not None:
                    inst.then_inc(chan_sems[op.chan], 16)
                elif op.signal:
                    inst.then_inc(sems[op.eng], 1)
            last = {}
            for op in per_eng[ename]:
                if op.chan is not None:
                    last[op.chan] = op.value
            for c, v in last.items():
                if waited.get(("c", c), 0) < v:
                    h.wait_ge(chan_sems[c], v)

        with nc.Block() as block:
            @block.tensor
            def _(e):
                run_engine("pe", e)

            @block.scalar
            def _(e):
                run_engine("act", e)

            @block.vector
            def _(e):
                run_engine("dve", e)

            @block.gpsimd
            def _(e):
                run_engine("pool", e)

            @block.sync
            def _(e):
                run_engine("sp", e)
        self.ops = []
        for b in self.bufs:
            b.w.clear()
            b.r.clear()
        self.bufs = []


def build():
    nc = bass.Bass("TRN2", target_bir_lowering=False)

    def din(name, shape, dt=F32):
        return nc.dram_tensor(name, list(shape), dt, kind="ExternalInput").ap()

    xT = din("xT", [NCH, 128, 8, CH])
    xTw = din("xTw", [4, 5, 128, 8, CH])
    xown = din("xown", [2048, D])
    w_in = din("w_in", [128, 8, 3080])
    w_out = din("w_out", [128, 8, D])
    wg = din("wg", [NE, 128, 8, 512])
    wu = din("wu", [NE, 128, 8, 512])
    wd = din("wd", [NE, 128, 4, D])
    wr = din("wr", [128, 8, 36])
    gvec = din("gvec", [128, 28])
    bfv = din("bfv", [8, 1])
    brv = din("brv", [128, 36])
    cbf = din("cbf", [128, 512], BF16)
    fmaskd = din("fmask", [128, 4, 512], BF16)
    dmaskd = din("dmask", [128, 10, 512], BF16)
    identf = din("identf", [128, 128])
    cosw = din("cosw", [4, 128, 5 * CH])
    sinw = din("sinw", [4, 128, 5 * CH])
    tabs = din("tabs", [128, 352])
    oh8d = din("oh8", [8, 64])
    y = nc.dram_tensor("y", [2048, D], F32, kind="ExternalOutput").ap()
    Kd = nc.dram_tensor("Kd", [4, 128, S_LEN], BF16, kind="Internal").ap()
    Vd = nc.dram_tensor("Vd", [4, 128, 64, 130], BF16, kind="Internal").ap()

    with ExitStack() as es:
        S = Sched(nc, es)

        def sbt(stack, name, shape, dt):
            return stack.enter_context(nc.sbuf_tensor(name, list(shape), dt))

        pbank = [es.enter_context(nc.psum_tensor("pb%d" % i, [128, 512], F32)) for i in range(7)]
        pbb = es.enter_context(nc.psum_tensor("pbb", [128, 1024], BF16))
        pbuf = [None] * 7
        ring = {}
        POOLS = {"all": [0, 1, 2, 3, 4, 5, 6], "r": [6], "raw": [0, 1, 2], "h": [3, 4], "m": [5, 6], "c1": [0, 1, 2, 3], "c2": [4, 5]}

        def new_pbufs():
            for i in range(7):
                pbuf[i] = S.buf("pb%d" % i)
            ring.clear()

        def nbank(pool="m"):
            lst = POOLS[pool]
            k = ring.get(pool, 0)
            ring[pool] = k + 1
            i = lst[k % len(lst)]
            return pbank[i], pbuf[i]


        class Pipe:
            def __init__(self):
                self.pending = []

            def push(self, stages):
                self.pending.append(list(stages))
                self.step()

            def step(self):
                for st in list(self.pending):
                    st.pop(0)()
                self.pending = [st for st in self.pending if st]

            def drain(self):
                while self.pending:
                    self.step()

        def mm(out, lhsT, rhs, start, stop, R, W):
            S.op("pe", lambda e: e.matmul(out, lhsT, rhs, start=start, stop=stop), R, W)

        def tr(out, in_, ident, R, W):
            S.op("pe", lambda e: e.transpose(out, in_, ident), R, W)

        def act(out, in_, func, R, W, bias=None, scale=None, accum_out=None):
            kw = {}
            if bias is not None:
                kw["bias"] = bias
            if scale is not None:
                kw["scale"] = scale
            if accum_out is not None:
                kw["accum_out"] = accum_out
            S.op("act", lambda e: e.activation(out=out, in_=in_, func=func, **kw), R, W)

        def ts(eng, out, in0, s1, op0, R, W, s2=None, op1=None):
            if op1 is None:
                S.op(eng, lambda e: e.tensor_scalar(out=out, in0=in0, scalar1=s1, scalar2=None, op0=op0), R, W)
            else:
                S.op(eng, lambda e: e.tensor_scalar(out=out, in0=in0, scalar1=s1, scalar2=s2, op0=op0, op1=op1), R, W)

        def tt(eng, out, in0, in1, op, R, W):
            S.op(eng, lambda e: e.tensor_tensor(out=out, in0=in0, in1=in1, op=op), R, W)

        def stt(out, in0, scalar, in1, op0, op1, R, W):
            S.op("dve", lambda e: e.scalar_tensor_tensor(out=out, in0=in0, scalar=scalar, in1=in1, op0=op0, op1=op1), R, W)

        def cp(eng, out, in_, R, W):
            S.op(eng, lambda e: e.tensor_copy(out=out, in_=in_), R, W)

        def recip(out, in_, R, W):
            S.op("dve", lambda e: e.reciprocal(out=out, in_=in_), R, W)

        def memset(eng, ap, val, W):
            S.op(eng, lambda e: e.memset(ap, val), (), W)

        cb = sbt(es, "cb", [128, 512], BF16)
        gv = sbt(es, "gv", [128, 28], F32)
        idf = sbt(es, "idf", [128, 128], F32)
        tab = sbt(es, "tab", [128, 352], F32)
        onesf = sbt(es, "onesf", [128, 128], F32)
        es_bc = ExitStack()
        oreg = sbt(es_bc, "oreg", [128, 16384], BF16)
        Otok = oreg[:, :].rearrange("p (a b) -> p a b", b=1024)
        ssq = sbt(es_bc, "ssq", [128, 16, 16], F32)
        ones_bf = cb[:, 0:128]
        bdiag = cb[:, 128:256]
        RTm = cb[:, 256:384]
        idb = cb[:, 384:512]
        g_mix = lambda c: gv[:, c:c + 1]
        g_ffn = lambda c: gv[:, 8 + c:9 + c]
        g_out = lambda c: gv[:, 16 + c:17 + c]
        g_qk = lambda i: gv[:, 24 + i:25 + i]
        deadf = lambda ti, kt: tab[:, ti * 64 + kt: ti * 64 + kt + 1]
        amask = lambda ti, kr: tab[:, 256 + ti * 4 + kr: 256 + ti * 4 + kr + 1]
        deadd = lambda ti, w: tab[:, 272 + ti * 20 + w: 272 + ti * 20 + w + 1]

        def load_weights(stack, Bc, name, col_ranges, stg, stg_b, Wt, Wb, gain_fn, src, chan):
            halves = [(stg[:, :, 0:128], S.buf(name + "h0"), chan + "a"), (stg[:, :, 128:256], S.buf(name + "h1"), chan + "b")]
            off = 0
            k = 0
            for (c0, c1) in col_ranges:
                pos = c0
                while pos < c1:
                    n = min(128, c1 - pos)
                    sv, sB, ch = halves[k % 2]
                    S.dma("sp", sv[:, :, 0:n], src[:, :, pos:pos + n], (), [sB], chan=ch)
                    for c in range(8):
                        if c % 2 == 0:
                            ts("dve", Wt[:, c, off:off + n], sv[:, c, 0:n], gain_fn(c), ALU.mult, [sB, Bc], [Wb])
                        else:
                            act(Wt[:, c, off:off + n], sv[:, c, 0:n], AF.Copy, [sB, Bc], [Wb], scale=gain_fn(c))
                    off += n
                    pos += n
                    k += 1
            return off

        def chunk_pre(P, src_ap, slot, need_r8, part="ab", cast_eng="act", sq_eng="dve"):
            xf, xb, xsq = P["xf"][slot], P["xb"][slot], P["xsq"][slot]
            xfB, xbB, xsqB = P["xfB"][slot], P["xbB"][slot], P["xsqB"][slot]
            if "a" in part:
                xch = "xf%d" % (slot % P["nxf"])
                S.dma("sp", xf[:, 0:4, :], src_ap[:, 0:4, :], (), [xfB], chan=xch)
                S.dma("sp", xf[:, 4:8, :], src_ap[:, 4:8, :], (), [xfB], chan=xch)
                if cast_eng == "act":
                    act(xb[:].rearrange("p c t -> p (c t)"), xf[:].rearrange("p c t -> p (c t)"), AF.Copy, [xfB], [xbB])
                else:
                    for hh in range(4):
                        cp("pool", xb[:, 2 * hh:2 * hh + 2, :], xf[:, 2 * hh:2 * hh + 2, :], [xfB], [xbB])
                if sq_eng == "act":
                    act(xsq[:].rearrange("p c t -> p (c t)"), xf[:].rearrange("p c t -> p (c t)"), AF.Square, [xfB], [xsqB])
                else:
                    for hh in range(2):
                        tt("dve", xsq[:, 4 * hh:4 * hh + 4, :], xf[:, 4 * hh:4 * hh + 4, :], xf[:, 4 * hh:4 * hh + 4, :], ALU.mult,
                           [xfB], [xsqB])
            if "b" not in part:
                return
            bk, bb = nbank()
            for c in range(8):
                mm(bk[:, :], ones_bf, xsq[:, c, :], c == 0, c == 7, [xsqB, P["cB"]], [bb])
            et, etB = P["et"][slot], P["etB"][slot]
            ts("dve", et[:, :], bk[:, :], EPS / 1024.0, ALU.mult, [bb], [etB], s2=EPS * EPS, op1=ALU.add)
            if need_r8:
                r8, r8B = P["r8"][slot], P["r8B"][slot]
                ts("dve", r8[0:8, :], bk[0:8, :], 1.0 / 1024.0, ALU.mult, [bb], [r8B], s2=EPS, op1=ALU.add)
                act(r8[0:8, :], r8[0:8, :], AF.Ln, [r8B], [r8B])
                act(r8[0:8, :], r8[0:8, :], AF.Exp, [r8B], [r8B], scale=-0.5)
            bk2, bb2 = nbank()
            for t4 in range(4):
                for c in range(8):
                    mm(bk2[:, 2 * t4:2 * t4 + 2], xsq[:, c, t4 * 128:(t4 + 1) * 128], cb[:, 0:2], c == 0, c == 7,
                       [xsqB, P["cB"]], [bb2])
            rt, rtB = P["rt"][slot], P["rtB"][slot]
            ts("dve", rt[:, 0:8], bk2[:, 0:8], 1.0 / 1024.0, ALU.mult, [bb2], [rtB], s2=EPS, op1=ALU.add)
            act(rt[:, 0:8], rt[:, 0:8], AF.Ln, [rtB], [rtB])
            act(rt[:, 0:8], rt[:, 0:8], AF.Exp, [rtB], [rtB], scale=-0.5)

        def norm_stages(P, Wt, WB, colofs, xb, xbB, et, etB, gain_ap, rope, out_ap, outB, k, final=None):
            ksq, t, t2, kn = P["nt"][k % 3]
            ksqB, tB, t2B, knB = P["ntB"][k % 3]
            hold = {}

            def s0():
                bk, bb = nbank("raw")
                hold["raw"] = (bk, bb)
                for c in range(8):
                    mm(bk[:, :], Wt[:, c, colofs:colofs + 128], xb[:, c, :], c == 0, c == 7, [WB, xbB], [bb])
                act(ksq[:, :], bk[:, :], AF.Square, [bb], [ksqB])

            def s1():
                bk, bb = hold["raw"]
                b2, bb2 = nbank("h")
                mm(b2[:, :], bdiag, ksq[:, :], True, True, [ksqB, P["cB"]], [bb2])
                stt(t[:, :], b2[:, :], 1.0 / 64.0, et[:, :], ALU.mult, ALU.add, [bb2, etB], [tB])
                act(t[:, :], t[:, :], AF.Ln, [tB], [tB])
                act(t[:, :], t[:, :], AF.Exp, [tB], [tB], scale=-0.5)
                if rope is None:
                    stt(out_ap, bk[:, :], gain_ap, t[:, :], ALU.mult, ALU.mult, [bb, tB, P["cB"]], [outB])
                    if final is not None:
                        final()
                else:
                    stt(kn[:, :], bk[:, :], gain_ap, t[:, :], ALU.mult, ALU.mult, [bb, tB, P["cB"]], [knB])

            def s2():
                cos_ap, sin_ap, csB = rope
                b3, bb3 = nbank("h")
                mm(b3[:, :], RTm, kn[:, :], True, True, [knB, P["cB"]], [bb3])
                tt("pool", t[:, :], kn[:, :], cos_ap, ALU.mult, [knB, csB], [tB])
                tt("dve", t2[:, :], b3[:, :], sin_ap, ALU.mult, [bb3, csB], [t2B])
                tt("pool", out_ap, t[:, :], t2[:, :], ALU.add, [tB, t2B], [outB])
                if final is not None:
                    final()

            return [s0, s1] if rope is None else [s0, s1, s2]

        def alloc_proj(stack, pre, need_r8, nxb=2, nxf=2):
            P = {}
            P["cB"] = constB[0]
            xfl = [sbt(stack, pre + "xf%d" % i, [128, 8, CH], F32) for i in range(nxf)]
            P["xf"] = [xfl[0], xfl[-1]]
            xbl = [sbt(stack, pre + "xb%d" % i, [128, 8, CH], BF16) for i in range(nxb)]
            xsl = [sbt(stack, pre + "xsq%d" % i, [128, 8, CH], BF16) for i in range(nxb)]
            P["xb"] = [xbl[0], xbl[-1]]
            P["xsq"] = [xsl[0], xsl[-1]]
            P["et"] = [sbt(stack, pre + "et%d" % i, [128, CH], F32) for i in range(2)]
            P["rt"] = [sbt(stack, pre + "rt%d" % i, [128, 8], F32) for i in range(2)]
            for nme in ("et", "rt"):
                P[nme + "B"] = [S.buf(pre + nme + "B%d" % i) for i in range(2)]
            xfbl = [S.buf(pre + "xfB%d" % i) for i in range(nxf)]
            P["xfB"] = [xfbl[0], xfbl[-1]]
            P["nxf"] = nxf
            for nme in ("xb", "xsq"):
                bl = [S.buf(pre + nme + "B%d" % i) for i in range(nxb)]
                P[nme + "B"] = [bl[0], bl[-1]]
            if need_r8:
                P["r8"] = [sbt(stack, pre + "r8%d" % i, [8, CH], F32) for i in range(2)]
                P["r8B"] = [S.buf(pre + "r8B%d" % i) for i in range(2)]
            P["nt"] = [(sbt(stack, pre + "ksq%d" % i, [128, CH], BF16), sbt(stack, pre + "t%d" % i, [128, CH], F32),
                        sbt(stack, pre + "t2%d" % i, [128, CH], F32), sbt(stack, pre + "kn%d" % i, [128, CH], BF16))
                       for i in range(3)]
            P["ntB"] = [tuple(S.buf(pre + "ntB%d_%d" % (i, q)) for q in range(4)) for i in range(3)]
            return P

        constB = [None]

        def load_consts():
            constB[0] = S.buf("const")
            cB = constB[0]
            S.dma("sp", cb[:, :], cbf[:, :], (), [cB], chan="c0")
            S.dma("sp", gv[:, :], gvec[:, :], (), [cB], chan="c0")
            S.dma("sp", idf[:, :], identf[:, :], (), [cB], chan="c0")
            S.dma("sp", tab[:, :], tabs[:, :], (), [cB], chan="c0")
            memset("pool", onesf[:, :], 1.0, [cB])

        ucnt = {"n": 0}

        def attention(steps, ptile, ptB, scr, scrB):
            n_steps = len(steps)
            NS = 5
            LA = 3

            def issue_s(n):
                st = steps[n]
                bk, bb = pbank[2 + n % NS], pbuf[2 + n % NS]
                st["sb"] = (bk, bb)
                mm(bk[:, st["qlo"]:512], st["k"], st["q"], True, True, st["kqR"], [bb])

            for n in range(min(LA, n_steps)):
                issue_s(n)
            for n in range(n_steps):
                if n + LA < n_steps:
                    issue_s(n + LA)
                st = steps[n]
                if st["first"]:
                    ucnt["n"] += 1
                u = ucnt["n"] % 2
                ob, obB = pbank[u], pbuf[u]
                bk, bb = st["sb"]
                qlo = st["qlo"]
                pt, pB = ptile[n % 3], ptB[n % 3]
                act(pt[:, qlo:512], bk[:, qlo:512], AF.Exp, [bb] + st["biasR"], [pB], bias=st["bias"], scale=0.125)
                if st["mask"] is not None:
                    st["mask"](pt, pB, qlo)
                for qb in range(qlo // 128, 4):
                    start = bool(st["first"] and qb == 0)
                    last = st["last"](qb)
                    S.op("pe", (lambda o, l, r, a, b: (lambda e: e.matmul(o, l, r, start=a, stop=b, skip_group_check=True)))(
                        ob[:, qb * 65:(qb + 1) * 65], pt[:, qb * 128:(qb + 1) * 128], st["v"], start, last),
                        [pB] + st["vR"], [obB])
                if st["final"]:
                    ti, hglob = st["unit"]
                    ov = ob[:, 0:260].rearrange("p (q e) -> p q e", e=65)
                    rl4 = scr[0][:, 4 * u:4 * u + 4]
                    recip(rl4, ov[:, :, 64], [obB], [scrB])
                    tt("dve", Otok[:, ti * 4:(ti + 1) * 4, hglob * 64:(hglob + 1) * 64], ov[:, :, 0:64],
                       rl4.unsqueeze(2).broadcast_to([128, 4, 64]), ALU.mult, [obB, scrB], [OtokB[0]])

        OtokB = [None]

        new_pbufs()
        load_consts()
        cB = constB[0]
        OtokB[0] = S.buf("Otok")
        with ExitStack() as pw:
            P = alloc_proj(pw, "w", False, nxb=2, nxf=1)
            stg = sbt(pw, "w_stg", [128, 8, 256], F32)
            stgB = S.buf("stgw")
            Ww = sbt(pw, "Ww", [128, 8, 1536], BF16)
            WwB = S.buf("Ww")
            load_weights(pw, cB, "ww", [(1536, 2048), (2048, 2560), (2560, 3072)], stg, stgB, Ww, WwB, g_mix, w_in, "stgw")
            dm = sbt(pw, "dm", [128, 10, 512], BF16)
            S.dma("sp", dm[:].rearrange("p a b -> p (a b)"), dmaskd.rearrange("p a b -> p (a b)"), (), [cB], chan="c0")
            KTw = sbt(pw, "KTw", [128, 4, 5 * CH], BF16)
            KTwB = S.buf("KTw")
            Vw = sbt(pw, "Vw", [128, 20, 8 * 65], BF16)
            VwB = S.buf("Vw")
            memset("pool", Vw[:].rearrange("p a b -> p (a b)"), 1.0, [VwB])
            QTd = sbt(pw, "QTd", [128, 4, CH], BF16)
            QTdB = S.buf("QTd")
            cs = [(sbt(pw, "cos%d" % i, [128, CH], F32), sbt(pw, "sin%d" % i, [128, CH], F32)) for i in range(2)]
            csB = [S.buf("csB%d" % i) for i in range(2)]
            ptile = [sbt(pw, "wpt%d" % i, [128, 512], BF16) for i in range(3)]
            ptB = [S.buf("wptB%d" % i) for i in range(3)]
            scr = (sbt(pw, "wrl", [128, 8], F32), sbt(pw, "wjunk", [128, 64], F32))
            scrB = S.buf("wscr")
            midx = [0, 1, 2, 3] + [4] * 8 + [5, 6, 7, 8, 9] + [9, 9, 9]
            kcount = 0
            def w_pre(g, part):
                ti_, wc_ = g // 5, g % 5
                sl = g % 2
                chunk_pre(P, xTw[ti_, wc_], sl, False, part=part)
                if "a" in part:
                    S.dma("sp", cs[sl][0][:, :], cosw[ti_, :, wc_ * CH:(wc_ + 1) * CH], (), [csB[sl]], chan="cs%d" % sl)
                    S.dma("sp", cs[sl][1][:, :], sinw[ti_, :, wc_ * CH:(wc_ + 1) * CH], (), [csB[sl]], chan="cs%d" % sl)

            w_pre(0, "ab")
            for ti in range(4):
                for wc in range(5):
                    g = ti * 5 + wc
                    slot = g % 2
                    xb, xbB = P["xb"][slot], P["xbB"][slot]
                    et, etB = P["et"][slot], P["etB"][slot]
                    rt, rtB = P["rt"][slot], P["rtB"][slot]
                    rope = (cs[slot][0][:, :], cs[slot][1][:, :], csB[slot])
                    if wc == 0:
                        pipe = Pipe()

                    def vproj(t4, xb=xb, xbB=xbB, rt=rt, rtB=rtB, wc=wc):
                        bk, bb = nbank("m")
                        for c in range(8):
                            mm(bk[:, :], xb[:, c, t4 * 128:(t4 + 1) * 128], Ww[:, c, 1024:1536], c == 0, c == 7, [xbB, WwB], [bb])
                        act(Vw[:, wc * 4 + t4, :].rearrange("p (h e) -> p h e", e=65)[:, :, 0:64],
                            bk[:, :].rearrange("p (h e) -> p h e", e=64), AF.Copy, [bb, rtB], [VwB],
                            scale=rt[:, 2 * t4:2 * t4 + 1])

                    for hp in range(4):
                        if hp == 2 and g + 1 < 20:
                            w_pre(g + 1, "a")
                        pipe.push(norm_stages(P, Ww, WwB, 512 + hp * 128, xb, xbB, et, etB, g_qk(3), rope,
                                              KTw[:, hp, wc * CH:(wc + 1) * CH], KTwB, kcount))
                        kcount += 1
                        vproj(hp)
                    if wc == 4:
                        for hp in range(4):
                            pipe.push(norm_stages(P, Ww, WwB, hp * 128, xb, xbB, et, etB, g_qk(2), rope,
                                                  QTd[:, hp, :], QTdB, kcount))
                            kcount += 1
                    if wc == 4:
                        pipe.drain()
                    if g + 1 < 20:
                        w_pre(g + 1, "b")
                steps = []
                for h in range(8):
                    hp, half = h // 2, h % 2
                    p0 = 64 * half
                    for w in range(20):
                        r = w - 16
                        qlo = max(0, r) * 128
                        mi = midx[w]

                        def mk(pt, pB, qlo, mi=mi, w=w):
                            eng = "dve"
                            tt(eng, pt[:, qlo:512], pt[:, qlo:512], dm[:, mi, 0:512 - qlo], ALU.mult, [pB, cB], [pB])
                        steps.append({
                            "qlo": qlo,
                            "k": KTw[p0:p0 + 64, hp, w * 128:(w + 1) * 128],
                            "q": QTd[p0:p0 + 64, hp, qlo:512],
                            "kqR": [KTwB, QTdB],
                            "bias": deadd(ti, w),
                            "biasR": [cB],
                            "v": Vw[:, w, h * 65:(h + 1) * 65],
                            "vR": [VwB],
                            "first": w == 0,
                            "last": (lambda qb, w=w: w == 16 + qb),
                            "final": w == 19,
                            "unit": (ti, 8 + h),
                            "mask": mk,
                        })
                attention(steps, ptile, ptB, scr, scrB)
            S.flush()

        es_ab = ExitStack()
        QTf = sbt(es_ab, "QTf", [128, 4, 2048], BF16)
        biasT = sbt(es_ab, "biasT", [128, 4, 64, 8], F32)
        new_pbufs()
        load_consts()
        cB = constB[0]
        QTfB = S.buf("QTf")
        biasB = S.buf("biasT")
        KdB = S.buf("Kd")
        VdB = S.buf("Vd")
        with ExitStack() as pa:
            P = alloc_proj(pa, "a", True)
            stg = sbt(pa, "a_stg", [128, 8, 256], F32)
            stgB = S.buf("stg")
            Wa = sbt(pa, "Wa", [128, 8, 1544], BF16)
            WaB = S.buf("Wa")
            load_weights(pa, cB, "wa", [(512, 1024), (1024, 1536), (3072, 3080), (0, 512)], stg, stgB, Wa, WaB,
                         g_mix, w_in, "stg")
            cc = sbt(pa, "cc", [8, 2, CH], F32)
            cTB = S.buf("cT")
            cmid8 = sbt(pa, "cmid8", [8, 16], F32)
            ctok = sbt(pa, "ctok", [128, 64, 8], F32)
            ctokB = S.buf("ctok")
            bf_sb = sbt(pa, "bf_sb", [8, 1], F32)
            oh8 = sbt(pa, "oh8s", [8, 64], F32)
            S.dma("sp", bf_sb[:, :], bfv[:, :], (), [cB], chan="c0")
            S.dma("sp", oh8[:, :], oh8d[:, :], (), [cB], chan="c0")
            ktile = [sbt(pa, "a_kt%d" % i, [128, CH], BF16) for i in range(4)]
            ktB = [S.buf("a_ktB%d" % i) for i in range(4)]
            vsb = [sbt(pa, "a_v%d" % i, [128, 4, 4, 130], BF16) for i in range(2)]
            vsbB = [S.buf("a_vB%d" % i) for i in range(2)]
            for i in range(2):
                memset("pool", vsb[i][:].rearrange("p a b c -> p (a b c)"), 1.0, [vsbB[i]])
            fz = sbt(pa, "a_fz", [8, 3, CH], F32)
            fzB = S.buf("fz")
            kcount = 0
            deferred = []
            def a_pre(kc_, part):
                if kc_ < NCH:
                    chunk_pre(P, xT[kc_], kc_ % 2, True, part=part)
                else:
                    chunk_pre(P, xTw[kc_ - NCH, 4], kc_ % 2, False, part=part)

            a_pre(0, "ab")
            for kc in range(NCH + 4):
                slot = kc % 2
                is_q = kc >= NCH
                if is_q:
                    while deferred:
                        deferred.pop(0)()
                xb, xbB = P["xb"][slot], P["xbB"][slot]
                et, etB = P["et"][slot], P["etB"][slot]
                rt, rtB = P["rt"][slot], P["rtB"][slot]
                if kc == 0:
                    pipe = Pipe()
                if is_q:
                    ti = kc - NCH
                    for hp in range(4):
                        if hp == 2 and kc + 1 < NCH + 4:
                            a_pre(kc + 1, "a")
                        pipe.push(norm_stages(P, Wa, WaB, 1032 + hp * 128, xb, xbB, et, etB, g_qk(0), None,
                                              QTf[:, hp, ti * CH:(ti + 1) * CH], QTfB, kcount))
                        kcount += 1
                    if kc + 1 < NCH + 4:
                        a_pre(kc + 1, "b")
                    else:
                        pipe.drain()
                    continue
                vs = kc % 2

                def vproj(t4, xb=xb, xbB=xbB, rt=rt, rtB=rtB, vs=vs):
                    bk, bb = nbank("m")
                    for c in range(8):
                        mm(bk[:, :], xb[:, c, t4 * 128:(t4 + 1) * 128], Wa[:, c, 512:1024], c == 0, c == 7, [xbB, WaB], [bb])
                    act(vsb[vs][:, :, t4, :].rearrange("p a (h e) -> p a h e", e=65)[:, :, :, 0:64],
                        bk[:, :].rearrange("p (a h e) -> p a h e", a=4, e=64), AF.Copy, [bb, rtB], [vsbB[vs]],
                        scale=rt[:, 2 * t4:2 * t4 + 1])

                for hp in range(4):
                    if hp == 2:
                        while deferred:
                            deferred.pop(0)()
                        if kc + 1 < NCH + 4:
                            a_pre(kc + 1, "a")
                    ks = kcount % 4

                    def spill(hp=hp, ks=ks, kc=kc):
                        S.dma("pool", Kd[hp, :, kc * CH:(kc + 1) * CH], ktile[ks][:, :], [ktB[ks]], [KdB], chan="ks%d" % ks)
                    pipe.push(norm_stages(P, Wa, WaB, hp * 128, xb, xbB, et, etB, g_qk(1), None,
                                          ktile[ks][:, :], ktB[ks], kcount, final=spill))
                    kcount += 1
                    vproj(hp)
                for hp in range(4):
                    S.dma("pool", Vd[hp, :, kc * 4:(kc + 1) * 4, :], vsb[vs][:, hp, :, :],
                          [vsbB[vs]], [VdB], chan="vs%d" % vs)
                bk, bb = nbank()
                for c in range(8):
                    mm(bk[0:8, :], Wa[:, c, 1024:1032], xb[:, c, :], c == 0, c == 7, [WaB, xbB], [bb])
                r8, r8B = P["r8"][slot], P["r8B"][slot]
                z, mneg, na = fz[:, 0, :], fz[:, 1, :], fz[:, 2, :]
                tt("dve", z, bk[0:8, :], r8[0:8, :], ALU.mult, [bb, r8B], [fzB])
                ts("dve", z, z, bf_sb[:, 0:1], ALU.add, [fzB, cB], [fzB])
                ts("dve", mneg, z, 0.0, ALU.min, [fzB], [fzB])
                ts("dve", na, z, -1.0, ALU.mult, [fzB], [fzB])
                tt("dve", na, na, z, ALU.min, [fzB], [fzB])
                act(na, na, AF.Exp, [fzB], [fzB])
                act(na, na, AF.Ln, [fzB], [fzB], bias=1.0)
                tt("dve", z, mneg, na, ALU.subtract, [fzB], [fzB])
                init = 0.0 if kc == 0 else cc[0:8, (kc - 1) % 2, CH - 1:CH]
                S.op("dve", (lambda o, d0, d1, ini: (lambda e: e.tensor_tensor_scan(out=o, data0=d0, data1=d1, initial=ini,
                                                                                     op0=ALU.mult, op1=ALU.add)))(
                    cc[0:8, kc % 2, :], onesf[0:8, 0:1].broadcast_to([8, CH]), z, init), [fzB, cTB, cB], [cTB])
                cp("dve", cmid8[:, kc:kc + 1], cc[0:8, kc % 2, 255:256], [cTB], [cTB])

                def ctok_tr(kc=kc):
                    bk, bb = nbank()
                    for t4 in range(4):
                        tr(bk[:, 8 * t4:8 * t4 + 8], cc[0:8, kc % 2, t4 * 128:(t4 + 1) * 128], idf[0:8, 0:8], [cTB, cB], [bb])
                    cp("dve", ctok[:, kc * 4:(kc + 1) * 4, :].rearrange("p a b -> p (a b)"), bk[:, 0:32], [bb], [ctokB])
                deferred.append(ctok_tr)
                if kc + 1 < NCH + 4:
                    a_pre(kc + 1, "b")
            crw = sbt(pa, "crw", [8, 64], F32)
            crB = S.buf("cr")
            cmid = cmid8[:, :].unsqueeze(1).broadcast_to([8, 4, 16])
            tt("dve", crw[:, :].rearrange("p (a b) -> p a b", b=16), oh8[:, :].rearrange("p (a b) -> p a b", b=16),
               cmid, ALU.mult, [cTB, cB], [crB])
            cr4 = sbt(pa, "cr4", [8, 4], F32)
            S.op("dve", (lambda o, i_: (lambda e: e.tensor_reduce(out=o, in_=i_, axis=AX.X, op=ALU.add)))(
                cr4[:, :], crw[:, :].rearrange("p (a b) -> p a b", b=16)), [crB], [crB])
            rd = sbt(pa, "rd", [8, 4, 8], F32)
            for ti in range(4):
                ts("dve", rd[:, ti, :], idf[0:8, 0:8], cr4[:, ti:ti + 1], ALU.mult, [crB, cB], [crB])
            bk, bb = nbank()
            mm(bk[:, 0:32], onesf[0:8, :], rd[:].rearrange("p a b -> p (a b)"), True, True, [crB, cB], [bb])
            crb = sbt(pa, "crb", [128, 32], F32)
            cp("dve", crb[:, :], bk[:, 0:32], [bb], [crB])
            for ti in range(4):
                crb_b = crb[:, ti * 8:(ti + 1) * 8].unsqueeze(1).broadcast_to([128, 64, 8])
                tt("dve", biasT[:, ti, :, :], crb_b, ctok[:, :, :], ALU.subtract, [crB, ctokB], [biasB])
                dead_b = tab[:, ti * 64:(ti + 1) * 64].unsqueeze(2).broadcast_to([128, 64, 8])
                tt("dve", biasT[:, ti, :, :], biasT[:, ti, :, :], dead_b, ALU.add, [biasB, cB], [biasB])
            S.flush()

        new_pbufs()
        load_consts()
        cB = constB[0]
        OtokB[0] = S.buf("Otok")
        QTfB = S.buf("QTf")
        biasB = S.buf("biasT")
        with ExitStack() as pbk:
            KTp = [sbt(pbk, "KTp%d" % i, [128, S_LEN], BF16) for i in range(2)]
            V2 = [sbt(pbk, "V2%d" % i, [128, 64, 130], BF16) for i in range(2)]
            KTpB = [S.buf("KTpB%d" % i) for i in range(2)]
            V2B = [S.buf("V2B%d" % i) for i in range(2)]
            fm = sbt(pbk, "fm", [128, 4, 512], BF16)
            S.dma("sp", fm[:].rearrange("p a b -> p (a b)"), fmaskd.rearrange("p a b -> p (a b)"), (), [cB], chan="c0")
            ptile = [sbt(pbk, "pt%d" % i, [128, 512], BF16) for i in range(3)]
            ptB = [S.buf("ptB%d" % i) for i in range(3)]
            scr = (sbt(pbk, "rl", [128, 8], F32), sbt(pbk, "junk", [128, 64], F32))
            scrB = S.buf("scr")

            def load_pair(hp):
                s_ = hp % 2
                for q4 in range(4):
                    S.dma("sp", KTp[s_][:, q4 * 2048:(q4 + 1) * 2048], Kd[hp, :, q4 * 2048:(q4 + 1) * 2048], (), [KTpB[s_]],
                          chan="kl%d" % s_)
                    S.dma("sp", V2[s_][:, q4 * 16:(q4 + 1) * 16, :], Vd[hp, :, q4 * 16:(q4 + 1) * 16, :], (), [V2B[s_]],
                          chan="vl%d" % s_)

            steps = []
            load_pair(0)
            for hp in range(4):
                s_ = hp % 2
                for half in range(2):
                    h = hp * 2 + half
                    p0 = 64 * half
                    for ti in range(4):
                        nkt = 16 * (ti + 1)
                        for kt in range(nkt):
                            kcr = kt // 4 - 4 * ti
                            d = kt % 4
                            st = {
                                "qlo": 0,
                                "k": KTp[s_][p0:p0 + 64, kt * 128:(kt + 1) * 128],
                                "q": QTf[p0:p0 + 64, hp, ti * CH:(ti + 1) * CH],
                                "kqR": [KTpB[s_], QTfB],
                                "bias": biasT[:, ti, kt, h:h + 1],
                                "biasR": [biasB],
                                "v": V2[s_][:, kt, half * 65:(half + 1) * 65],
                                "vR": [V2B[s_]],
                                "first": kt == 0,
                                "last": (lambda qb, kt=kt, nkt=nkt: kt == nkt - 1),
                                "final": kt == nkt - 1,
                                "unit": (ti, h),
                                "mask": None,
                            }
                            if kcr >= 0:
                                def mk(pt, pB, qlo, d=d, ti=ti, kcr=kcr):
                                    stt(pt[:, :], fm[:, d, :], amask(ti, kcr), pt[:, :], ALU.max, ALU.mult, [pB, cB], [pB])
                                st["mask"] = mk
                            steps.append(st)
            per_pair = len(steps) // 4
            for hp in range(4):
                if hp + 1 < 4:
                    load_pair(hp + 1)
                attention(steps[hp * per_pair:(hp + 1) * per_pair], ptile, ptB, scr, scrB)
            S.flush()
        es_ab.close()

        new_pbufs()
        load_consts()
        cB = constB[0]
        OtokB[0] = S.buf("Otok")
        with ExitStack() as pc:
            hsb = sbt(pc, "hsb", [128, 16, D], F32)
            hB = [S.buf("hB%d" % i) for i in range(16)]
            hnT = sbt(pc, "hnT", [128, 8, 2048], BF16)
            hnTB = S.buf("hnT")
            comb = sbt(pc, "comb", [128, 16, NE], F32)
            combB = S.buf("comb")
            br_sb = sbt(pc, "br_sb", [128, 36], F32)
            S.dma("sp", br_sb[:, :], brv[:, :], (), [cB], chan="c0")
            with ExitStack() as pcc:
                stg = sbt(pcc, "c_stg", [128, 8, 256], F32)
                stgB = S.buf("stgc")
                Wo = sbt(pcc, "Wo", [128, 8, D], BF16)
                WoB = S.buf("Wo")
                load_weights(pcc, cB, "wo", [(0, 1024)], stg, stgB, Wo, WoB, g_out, w_out, "stgc")
                wrs = sbt(pcc, "wrs", [128, 8, 36], F32)
                S.dma("sp", wrs[:, :, :], wr[:, :, :], (), [cB], chan="c0")
                ro = sbt(pcc, "ro", [128, 32], F32)
                roB = S.buf("ro")
                rjunk = sbt(pcc, "rjunk", [128, 512], BF16)
                rjB = S.buf("rjunk")
                for i in range(16):
                    for g in range(2):
                        act(rjunk[:, :], Otok[:, i, g * 512:(g + 1) * 512], AF.Square, [OtokB[0]], [rjB, roB],
                            accum_out=ro[:, 2 * i + g:2 * i + g + 1])
                ts("dve", ro[:, :], ro[:, :], 1.0 / 512.0, ALU.mult, [roB], [roB], s2=EPS, op1=ALU.add)
                act(ro[:, :], ro[:, :], AF.Ln, [roB], [roB])
                act(ro[:, :], ro[:, :], AF.Exp, [roB], [roB], scale=-0.5)
                for i in range(16):
                    for g in range(2):
                        act(Otok[:, i, g * 512:(g + 1) * 512], Otok[:, i, g * 512:(g + 1) * 512], AF.Copy,
                            [OtokB[0], roB], [OtokB[0]], scale=ro[:, 2 * i + g:2 * i + g + 1])
                oT = [sbt(pcc, "oT%d" % i, [128, 8, 128], BF16) for i in range(2)]
                oTB = [S.buf("oTB%d" % i) for i in range(2)]
                xo = [sbt(pcc, "xo%d" % i, [128, D], F32) for i in range(2)]
                xoB = [S.buf("xoB%d" % i) for i in range(2)]
                hs = [sbt(pcc, "hs%d" % i, [128, D], F32) for i in range(2)]
                hsB_ = [S.buf("hsB%d" % i) for i in range(2)]
                hTf = sbt(pcc, "hTf0", [128, 8, 128], F32)
                hTfB = S.buf("hTfB0")
                rh = sbt(pcc, "rh", [128, 16], F32)
                rhB = [S.buf("rh%d" % i) for i in range(16)]
                lg = sbt(pcc, "lg", [128, 16, 36], F32)
                lgBs = [S.buf("lg%d" % i) for i in range(16)]
                pbbB = S.buf("pbb")

                def c_stages(i):
                    s_ = i % 2

                    def s0():
                        S.dma("sp", xo[s_][:, :], xown[i * 128:(i + 1) * 128, :], (), [xoB[s_]], chan="xo%d" % s_)
                        for c in range(8):
                            tr(pbb[:, c * 128:(c + 1) * 128], Otok[:, i, c * 128:(c + 1) * 128], idb, [OtokB[0], cB], [pbbB])
                        act(oT[s_][:].rearrange("p a b -> p (a b)"), pbb[:, :], AF.Copy, [pbbB], [oTB[s_]])

                    def s1():
                        for hf in range(2):
                            bk, bb = nbank("c1")
                            for c in range(8):
                                mm(bk[:, :], oT[s_][:, c, :], Wo[:, c, hf * 512:(hf + 1) * 512], c == 0, c == 7, [oTB[s_], WoB], [bb])
                            tt("dve", hsb[:, i, hf * 512:(hf + 1) * 512], bk[:, :], xo[s_][:, hf * 512:(hf + 1) * 512], ALU.add,
                               [bb, xoB[s_]], [hB[i]])
                        act(hs[s_][:, :], hsb[:, i, :], AF.Square, [hB[i]], [hsB_[s_], rhB[i]], accum_out=rh[:, i:i + 1])
                        ts("dve", rh[:, i:i + 1], rh[:, i:i + 1], 1.0 / 1024.0, ALU.mult, [rhB[i]], [rhB[i]], s2=EPS, op1=ALU.add)
                        act(rh[:, i:i + 1], rh[:, i:i + 1], AF.Ln, [rhB[i]], [rhB[i]])
                        act(rh[:, i:i + 1], rh[:, i:i + 1], AF.Exp, [rhB[i]], [rhB[i]], scale=-0.5)
                        act(hs[s_][:, :], hsb[:, i, :], AF.Copy, [hB[i], rhB[i]], [hsB_[s_]], scale=rh[:, i:i + 1])

                    def s2():
                        for half8 in range(2):
                            bk, bb = nbank("c2")
                            for c4 in range(4):
                                c = half8 * 4 + c4
                                tr(bk[:, c4 * 128:(c4 + 1) * 128], hs[s_][:, c * 128:(c + 1) * 128], idf[:, :], [hsB_[s_], cB], [bb])
                            for c4 in range(4):
                                c = half8 * 4 + c4
                                ts("dve", hTf[:, c, :], bk[:, c4 * 128:(c4 + 1) * 128], g_ffn(c), ALU.mult, [bb, cB], [hTfB])
                        act(hnT[:, :, i * 128:(i + 1) * 128], hTf[:, :, :], AF.Copy, [hTfB], [hnTB])

                    def s3():
                        bk, bb = nbank("r")
                        for c in range(8):
                            mm(bk[:, 0:36], hTf[:, c, :], wrs[:, c, :], c == 0, c == 7, [hTfB, cB], [bb])
                        tt("dve", lg[:, i, :], bk[:, 0:36], br_sb[:, :], ALU.add, [bb, cB], [lgBs[i]])
                    return [s0, s1, s2, s3]

                pipe = Pipe()
                for i in range(16):
                    pipe.push(c_stages(i))
                pipe.drain()
                lgB = S.buf("lgall")
                S.op("dve", (lambda a: (lambda e: e.tensor_copy(out=a, in_=a)))(lg[:, 0, 0:1]), lgBs, [lgB])
                rw = sbt(pcc, "rw", [128, 16, 64], F32)
                rwB = S.buf("rw")
                zg = lg[:, :, 0:4]
                ze = lg[:, :, 4:36]
                gmax = rw[:, :, 0:1]
                S.op("dve", (lambda o, i_: (lambda e: e.tensor_reduce(out=o, in_=i_, axis=AX.X, op=ALU.max)))(
                    rw[:, :, 0], zg), [lgB], [rwB])
                goh = rw[:, :, 4:8]
                tt("dve", goh, zg, gmax.broadcast_to([128, 16, 4]), ALU.is_equal, [lgB, rwB], [rwB])
                gex = rw[:, :, 8:12]
                tt("dve", gex, zg, gmax.broadcast_to([128, 16, 4]), ALU.subtract, [lgB, rwB], [rwB])
                act(gex, gex, AF.Exp, [rwB], [rwB])
                S.op("dve", (lambda o, i_: (lambda e: e.tensor_reduce(out=o, in_=i_, axis=AX.X, op=ALU.add)))(
                    rw[:, :, 1], gex), [rwB], [rwB])
                pgt = rw[:, :, 1:2]
                recip(pgt, pgt, [rwB], [rwB])
                zs = rw[:, :, 16:24]
                ztmp = rw[:, :, 24:32]
                for g in range(4):
                    gsel = rw[:, :, 4 + g:5 + g].broadcast_to([128, 16, 8])
                    if g == 0:
                        tt("dve", zs, ze[:, :, 0:8], gsel, ALU.mult, [lgB, rwB], [rwB])
                    else:
                        tt("dve", ztmp, ze[:, :, g * 8:(g + 1) * 8], gsel, ALU.mult, [lgB, rwB], [rwB])
                        tt("dve", zs, zs, ztmp, ALU.add, [rwB], [rwB])
                m1 = rw[:, :, 2:3]
                S.op("dve", (lambda o, i_: (lambda e: e.tensor_reduce(out=o, in_=i_, axis=AX.X, op=ALU.max)))(
                    rw[:, :, 2], zs), [rwB], [rwB])
                oh1 = rw[:, :, 32:40]
                tt("dve", oh1, zs, m1.broadcast_to([128, 16, 8]), ALU.is_equal, [rwB], [rwB])
                zm = rw[:, :, 40:48]
                S.op("dve", (lambda o, a, b: (lambda e: e.scalar_tensor_tensor(out=o, in0=a, scalar=-1e30, in1=b, op0=ALU.mult,
                                                                                 op1=ALU.add)))(zm, oh1, zs), [rwB], [rwB])
                m2 = rw[:, :, 3:4]
                S.op("dve", (lambda o, i_: (lambda e: e.tensor_reduce(out=o, in_=i_, axis=AX.X, op=ALU.max)))(
                    rw[:, :, 3], zm), [rwB], [rwB])
                oh2 = rw[:, :, 48:56]
                tt("dve", oh2, zm, m2.broadcast_to([128, 16, 8]), ALU.is_equal, [rwB], [rwB])
                ee = rw[:, :, 12:13]
                tt("dve", ee, m2, m1, ALU.subtract, [rwB], [rwB])
                act(ee, ee, AF.Exp, [rwB], [rwB])
                den = rw[:, :, 13:14]
                ts("dve", den, ee, 1.0, ALU.add, [rwB], [rwB])
                recip(den, den, [rwB], [rwB])
                g1 = rw[:, :, 14:15]
                tt("dve", g1, den, pgt, ALU.mult, [rwB], [rwB])
                g2 = rw[:, :, 15:16]
                tt("dve", g2, g1, ee, ALU.mult, [rwB], [rwB])
                we = rw[:, :, 56:64]
                tt("dve", oh1, oh1, g1.broadcast_to([128, 16, 8]), ALU.mult, [rwB], [rwB])
                tt("dve", oh2, oh2, g2.broadcast_to([128, 16, 8]), ALU.mult, [rwB], [rwB])
                tt("dve", we, oh1, oh2, ALU.add, [rwB], [rwB])
                for g in range(4):
                    gsel = rw[:, :, 4 + g:5 + g].broadcast_to([128, 16, 8])
                    tt("dve", comb[:, :, g * 8:(g + 1) * 8], we, gsel, ALU.mult, [rwB], [combB])
                S.flush()

            new_pbufs()
            load_consts()
            cB = constB[0]
            hB = [S.buf("hB%d" % i) for i in range(16)]
            hnTB = S.buf("hnT")
            combB = S.buf("comb")
            with ExitStack() as pd:
                NST = 6
                stg4 = [sbt(pd, "d_stg%d" % i, [128, 1024], F32) for i in range(NST)]
                stg4B = [S.buf("d_stgB%d" % i) for i in range(NST)]
                wgb = [oreg[:, i * 4096:(i + 1) * 4096].rearrange("p (c n) -> p c n", n=512) for i in range(2)]
                wub = [oreg[:, 8192 + i * 4096:8192 + (i + 1) * 4096].rearrange("p (c n) -> p c n", n=512) for i in range(2)]
                wdb = [sbt(pd, "wdb%d" % i, [128, 4, D], BF16) for i in range(2)]
                wB = [[S.buf("wB%d_%d" % (k, i)) for i in range(2)] for k in range(3)]
                gs = [sbt(pd, "gs%d" % i, [128, 512], BF16) for i in range(2)]
                gsB = [S.buf("gsB%d" % i) for i in range(2)]
                HT = [sbt(pd, "HT%d" % i, [128, 4, 512], BF16) for i in range(2)]
                HTB = [S.buf("HTB%d" % i) for i in range(2)]
                sc = {"i": 0}

                def load_expert(e):
                    s_ = e % 2
                    pieces = []
                    for k, (src, dst) in enumerate(((wg, wgb), (wu, wub))):
                        v = src[e]
                        for q in range(4):
                            pieces.append((v[:, 2 * q:2 * q + 2, :], dst[s_][:, 2 * q:2 * q + 2, :], wB[k][s_], 2, 512))
                    v = wd[e]
                    for q in range(4):
                        pieces.append((v[:, q:q + 1, :], wdb[s_][:, q:q + 1, :], wB[2][s_], 1, 1024))
                    todo = []
                    for (sv, dv, db, a, b) in pieces:
                        def one(sv=sv, dv=dv, db=db, a=a):
                            i = sc["i"] % NST
                            sc["i"] += 1
                            S.dma("sp", stg4[i][:, :].rearrange("p (a b) -> p a b", a=a), sv, (), [stg4B[i]], chan="ds%d" % i)
                            cp("pool", dv, stg4[i][:, :].rearrange("p (a b) -> p a b", a=a), [stg4B[i]], [db])
                        todo.append(one)
                    return todo

                for one in load_expert(0):
                    one()
                ucount = 0
                for e in range(NE):
                    s_ = e % 2
                    todo = load_expert(e + 1) if e + 1 < NE else []
                    for tq in range(4):
                        hs_ = (e * 4 + tq) % 2
                        for fc in range(4):
                            if todo:
                                todo.pop(0)()
                            bg, bgB = nbank("all")
                            for c in range(8):
                                mm(bg[:, :], wgb[s_][:, c, fc * 128:(fc + 1) * 128], hnT[:, c, tq * 512:(tq + 1) * 512], c == 0, c == 7,
                                   [wB[0][s_], hnTB], [bgB])
                            bu, buB = nbank("all")
                            for c in range(8):
                                mm(bu[:, :], wub[s_][:, c, fc * 128:(fc + 1) * 128], hnT[:, c, tq * 512:(tq + 1) * 512], c == 0, c == 7,
                                   [wB[1][s_], hnTB], [buB])
                            g_ = ucount % 2
                            ucount += 1
                            act(gs[g_][:, :], bg[:, :], AF.Silu, [bgB], [gsB[g_]])
                            tt("dve", HT[hs_][:, fc, :], bu[:, :], gs[g_][:, :], ALU.mult, [buB, gsB[g_]], [HTB[hs_]])
                        for t4 in range(4):
                            i = tq * 4 + t4
                            for hf in range(2):
                                by, byB = nbank("all")
                                for fc in range(4):
                                    mm(by[:, :], HT[hs_][:, fc, t4 * 128:(t4 + 1) * 128], wdb[s_][:, fc, hf * 512:(hf + 1) * 512],
                                       fc == 0, fc == 3, [HTB[hs_], wB[2][s_]], [byB])
                                stt(hsb[:, i, hf * 512:(hf + 1) * 512], by[:, :], comb[:, i, e:e + 1], hsb[:, i, hf * 512:(hf + 1) * 512],
                                    ALU.mult, ALU.add, [byB, combB, hB[i]], [hB[i]])
                yB = S.buf("y")
                for i in range(16):
                    S.dma("sp", y[i * 128:(i + 1) * 128, :], hsb[:, i, :], [hB[i]], [yB], chan="yo%d" % (i % 2))
                S.flush()
        es_bc.close()
        print("ops per engine:", S.total, flush=True)
    return nc


def _consts():
    ones = np.ones((128, 128), np.float32)
    bd = np.zeros((128, 128), np.float32)
    bd[:64, :64] = 1.0
    bd[64:, 64:] = 1.0
    RT = np.zeros((128, 128), np.float32)
    for i in range(128):
        if i % 64 < 32:
            RT[i + 32, i] = -1.0
        else:
            RT[i - 32, i] = 1.0
    ident = np.eye(128, dtype=np.float32)
    cbf = np.concatenate([ones, bd, RT, ident], axis=1).astype(NBF)
    k = np.arange(128)[:, None]
    q = np.arange(512)[None, :]
    fmask = np.stack([(q >= 128 * d + k) for d in range(4)], axis=1).astype(np.float32).astype(NBF)

    def count(delta):
        c = ((delta >= 0) & (delta <= 128)).astype(np.float32)
        c += ((delta >= 0) & (delta <= 512) & (delta % 4 == 0)).astype(np.float32)
        c += ((delta >= 0) & (delta <= 2048) & (delta % 16 == 0)).astype(np.float32)
        return c
    rs = [-16, -15, -14, -13, -12, -4, -3, -2, -1, 0]
    dmask = np.stack([count(q - k - 128 * r) for r in rs], axis=1).astype(NBF)
    for r in range(-12, -4):
        assert np.array_equal(count(q - k - 128 * r), count(q - k + 128 * 12))
    return cbf, fmask, dmask, ident


def _rope_tables(pos):
    inv_freq = (1.0 / (np.float32(10000.0) ** (np.arange(0, 64, 2, dtype=np.float32) / np.float32(64)))).astype(np.float32)
    ang = (pos.astype(np.float32)[:, None] * inv_freq[None, :]).astype(np.float32)
    cos = np.cos(ang).astype(np.float32).T
    sin = np.sin(ang).astype(np.float32).T
    cosT = np.concatenate([cos, cos, cos, cos], axis=0)
    sinT = np.concatenate([sin, sin, sin, sin], axis=0)
    return np.ascontiguousarray(cosT), np.ascontiguousarray(sinT)


_NC_CACHE = {}


def kernel(x, norm_mix, w_in, b_forget, q_norm_fox, k_norm_fox, q_norm_dil, k_norm_dil,
           out_norm_fox, out_norm_dil, w_out, norm_ffn, w_router_group, b_router_group,
           w_router_expert, b_router_expert, w_gate, w_up, w_down):
    f = lambda a: np.ascontiguousarray(np.asarray(a, dtype=np.float32))
    x = f(x)
    cbf, fmask, dmask, ident = _consts()
    pc = lambda v: np.ascontiguousarray(f(v).reshape(8, 128).T)
    t2 = lambda v: np.tile(f(v).reshape(64), 2).reshape(128, 1)
    gvec = np.concatenate([pc(norm_mix[0]), pc(norm_ffn[0]),
                           pc(np.concatenate([f(out_norm_fox[0]), f(out_norm_dil[0])])),
                           t2(q_norm_fox[0]), t2(k_norm_fox[0]), t2(q_norm_dil[0]), t2(k_norm_dil[0])], axis=1)
    gvec = np.ascontiguousarray(gvec.astype(np.float32))
    bfv = f(b_forget[0]).reshape(8, 1)
    wr = np.ascontiguousarray(np.concatenate(
        [f(w_router_group[0]), f(w_router_expert[0]).transpose(1, 0, 2).reshape(D, 32)], axis=1))
    brv = np.ascontiguousarray(np.broadcast_to(
        np.concatenate([f(b_router_group[0]).reshape(4), f(b_router_expert[0]).reshape(32)])[None, :], (128, 36)))
    pm = lambda a, c: np.ascontiguousarray(a.reshape(a.shape[:-2] + (c, 128, a.shape[-1])).swapaxes(-3, -2))
    shared = {
        "w_in": pm(f(w_in[0]), 8), "w_out": pm(f(w_out[0]), 8), "wg": pm(f(w_gate[0]), 8), "wu": pm(f(w_up[0]), 8),
        "wd": pm(f(w_down[0]), 4), "wr": pm(wr, 8), "gvec": gvec, "bfv": bfv, "brv": brv, "cbf": cbf, "fmask": fmask, "dmask": dmask, "identf": ident,
    }
    xTs = [np.ascontiguousarray(x[b].T) for b in range(2)]
    cpm = lambda xt: np.ascontiguousarray(xt.reshape(8, 128, -1, CH).transpose(2, 1, 0, 3))
    xTc = [cpm(xTs[b]) for b in range(2)]
    in_maps = []
    for c in range(8):
        b, j = c // 4, c % 4
        own = own_chunks(j)
        xTw = np.zeros((4, D, 5 * CH), np.float32)
        cosw = np.zeros((4, 128, 5 * CH), np.float32)
        sinw = np.zeros((4, 128, 5 * CH), np.float32)
        tabs = np.zeros((128, 352), np.float32)
        oh8 = np.zeros((8, 4, 16), np.float32)
        for ti, ch in enumerate(own):
            lo = (ch - 4) * CH
            pos = np.arange(lo, lo + 5 * CH)
            valid = pos >= 0
            xTw[ti][:, valid] = xTs[b][:, pos[valid]]
            cw, sw = _rope_tables(np.maximum(pos, 0).astype(np.float32))
            cosw[ti], sinw[ti] = cw, sw
            for kt in range(64):
                tabs[:, ti * 64 + kt] = 0.0 if (kt // 4) <= ch else NEGB
            for kr in range(4):
                tabs[:, 256 + ti * 4 + kr] = 0.0 if (4 * ti + kr) == ch else 1.0
            for w in range(20):
                tabs[:, 272 + ti * 20 + w] = 0.0 if (lo + w * 128) >= 0 else NEGB
            oh8[:, ti, ch] = 1.0
        xown = np.concatenate([x[b, ch * CH:(ch + 1) * CH, :] for ch in own], axis=0)
        m = dict(shared)
        m.update({"xT": xTc[b], "xTw": np.stack([cpm(xTw[t_]) for t_ in range(4)]), "xown": np.ascontiguousarray(xown), "cosw": cosw, "sinw": sinw,
                  "tabs": tabs, "oh8": np.ascontiguousarray(oh8.reshape(8, 64))})
        in_maps.append(m)
    if "nc" not in _NC_CACHE:
        _NC_CACHE["nc"] = build()
    nc = _NC_CACHE["nc"]
    res = run_bass_kernel_spmd(nc, in_maps, core_ids=list(range(8)))
    out = np.zeros((2, S_LEN, D), np.float32)
    for c in range(8):
        b, j = c // 4, c % 4
        yc = np.asarray(res.results[c]["y"], dtype=np.float32)
        for ti, ch in enumerate(own_chunks(j)):
            out[b, ch * CH:(ch + 1) * CH, :] = yc[ti * CH:(ti + 1) * CH, :]
    return out
```
